# Optimizing a Trainium2 kernel written in Bass

```python
import jax, jax.numpy as jnp
from jax import lax
import numpy as np

D_MODEL = 1024
BATCH = 32
SEQ = 2048
DEPTH = 4

EPS = 1e-6
GRID_W = 64

POOL_WINDOWS = (2, 4, 8, 16)
N_POOL_GROUPS = 4
POOL_GROUP_DIM = D_MODEL // 8
POOL_DIM = N_POOL_GROUPS * POOL_GROUP_DIM

ATT_HEADS = 8
ATT_KV_HEADS = 2
ATT_HEAD_DIM = D_MODEL // 16
ATT_Q_DIM = ATT_HEADS * ATT_HEAD_DIM
ATT_KV_DIM = ATT_KV_HEADS * ATT_HEAD_DIM
Q_BLOCK = 128
ROPE_THETA = 10000.0

EVEN_IN_DIM = POOL_DIM + ATT_Q_DIM + 2 * ATT_KV_DIM
EVEN_MIX_DIM = POOL_DIM + ATT_Q_DIM

MLSTM_HEADS = 8
MLSTM_QK_DIM = D_MODEL // 16
MLSTM_V_DIM = D_MODEL // 8
MLSTM_QK_WIDTH = MLSTM_HEADS * MLSTM_QK_DIM
MLSTM_V_WIDTH = MLSTM_HEADS * MLSTM_V_DIM
N_GATES = 4 * MLSTM_HEADS
ODD_IN_DIM = 2 * MLSTM_QK_WIDTH + 2 * MLSTM_V_WIDTH + N_GATES
CONV_K = 5
CHUNK = 128

FFN_HIDDEN = -(-8 * D_MODEL // (3 * 256)) * 256

kernel_name = "hybrid_pool_gqa_mlstm_adaln_encoder"


def rms_norm(x):
    x32 = x.astype(jnp.float32)
    y = x32 * lax.rsqrt(jnp.mean(x32 * x32, axis=-1, keepdims=True) + EPS)
    return y.astype(x.dtype)


def modulate(h, shift, scale):
    return h * (1.0 + scale[:, None, :]) + shift[:, None, :]


def grid_rope(seq_len):
    rows = seq_len // GRID_W
    row = jnp.repeat(jnp.arange(rows), GRID_W).astype(jnp.float32)
    col = jnp.tile(jnp.arange(GRID_W), rows).astype(jnp.float32)
    n_freq = ATT_HEAD_DIM // 4
    inv_freq = ROPE_THETA ** (-jnp.arange(n_freq, dtype=jnp.float32) / n_freq)
    ang = jnp.concatenate([row[:, None] * inv_freq, col[:, None] * inv_freq], axis=-1)
    return jnp.cos(ang), jnp.sin(ang)


def apply_rope(x, cos, sin):
    x32 = x.astype(jnp.float32)
    x1, x2 = x32[..., 0::2], x32[..., 1::2]
    c, s = cos[None, :, None, :], sin[None, :, None, :]
    out = jnp.stack([x1 * c - x2 * s, x1 * s + x2 * c], axis=-1).reshape(x.shape)
    return out.astype(x.dtype)


def multiscale_pool(u):
    B, S, _ = u.shape
    u32 = u.astype(jnp.float32)
    csum = jnp.concatenate([jnp.zeros((B, 1, POOL_DIM), jnp.float32), jnp.cumsum(u32, axis=1)], axis=1)
    t = jnp.arange(S)
    means = []
    for g, w in enumerate(POOL_WINDOWS):
        lo = jnp.clip(t - w // 2, 0, S)
        hi = jnp.clip(t + w - w // 2, 0, S)
        cg = csum[..., g * POOL_GROUP_DIM:(g + 1) * POOL_GROUP_DIM]
        total = jnp.take(cg, hi, axis=1) - jnp.take(cg, lo, axis=1)
        count = (hi - lo).astype(jnp.float32)
        means.append(total / count[None, :, None])
    return (jnp.concatenate(means, axis=-1) - u32).astype(u.dtype)


def grid_attention(q, k, v, cos, sin, q_gain, k_gain):
    q = apply_rope(rms_norm(q) * q_gain, cos, sin)
    k = apply_rope(rms_norm(k) * k_gain, cos, sin)
    B, S, H, Dh = q.shape
    G = H // ATT_KV_HEADS
    nb = S // Q_BLOCK
    qb = q.reshape(B, nb, Q_BLOCK, ATT_KV_HEADS, G, Dh).transpose(1, 0, 3, 4, 2, 5)
    kt = k.transpose(0, 2, 1, 3)
    vt = v.transpose(0, 2, 1, 3)
    scale = Dh ** -0.5

    def block(q_blk):
        s = jnp.einsum('bkgqd,bksd->bkgqs', q_blk, kt).astype(jnp.float32) * scale
        p = jax.nn.softmax(s, axis=-1).astype(vt.dtype)
        return jnp.einsum('bkgqs,bksd->bkgqd', p, vt)

    o = lax.map(block, qb)
    return o.transpose(1, 0, 4, 2, 3, 5).reshape(B, S, H * Dh)


def pool_attention_mixer(h, w_in, w_pool, b_pool, pool_scale, q_gain, k_gain, w_out, cos, sin):
    B, S, _ = h.shape
    z = h @ w_in
    u = z[..., :POOL_DIM]
    q = z[..., POOL_DIM:POOL_DIM + ATT_Q_DIM]
    k = z[..., POOL_DIM + ATT_Q_DIM:POOL_DIM + ATT_Q_DIM + ATT_KV_DIM]
    v = z[..., POOL_DIM + ATT_Q_DIM + ATT_KV_DIM:]
    pooled = multiscale_pool(u).reshape(B, S, N_POOL_GROUPS, POOL_GROUP_DIM)
    a = (jnp.einsum('bsgc,gcd->bsgd', pooled, w_pool) + b_pool).reshape(B, S, POOL_DIM) * pool_scale
    att = grid_attention(q.reshape(B, S, ATT_HEADS, ATT_HEAD_DIM),
                         k.reshape(B, S, ATT_KV_HEADS, ATT_HEAD_DIM),
                         v.reshape(B, S, ATT_KV_HEADS, ATT_HEAD_DIM),
                         cos, sin, q_gain, k_gain)
    return jnp.concatenate([a, att], axis=-1) @ w_out


def centred_depthwise_conv(u, w, b):
    y = lax.conv_general_dilated(u, w[:, None, :].astype(u.dtype), window_strides=(1,),
                                 padding=[(CONV_K // 2, CONV_K // 2)],
                                 dimension_numbers=('NWC', 'WIO', 'NWC'),
                                 feature_group_count=u.shape[-1])
    return y + b


def mlstm_chunkwise(q, k, v, log_i, log_f):
    q, k, v = (a.astype(jnp.float32) for a in (q, k, v))
    B, H, S, Dk = q.shape
    Dv = v.shape[-1]
    nc = S // CHUNK

    def to_chunks(a):
        return jnp.moveaxis(a.reshape(B, H, nc, CHUNK, *a.shape[3:]), 2, 0)

    qc, kc, vc, ic, fc = (to_chunks(a) for a in (q, k, v, log_i, log_f))
    lower = jnp.tril(jnp.ones((CHUNK, CHUNK), bool))

    def step(carry, inp):
        C, n, m = carry
        qj, kj, vj, ij, fj = inp
        b = jnp.cumsum(fj, axis=-1)
        a = b + m[..., None]
        D = jnp.where(lower, b[..., :, None] - b[..., None, :] + ij[..., None, :], -jnp.inf)
        m_t = jnp.maximum(a, jnp.max(D, axis=-1))
        w_inter = jnp.exp(a - m_t)
        s = jnp.einsum('bhtd,bhsd->bhts', qj, kj) * jnp.exp(D - m_t[..., None])
        num = jnp.einsum('bhts,bhsv->bhtv', s, vj) + w_inter[..., None] * jnp.einsum('bhvd,bhtd->bhtv', C, qj)
        den = jnp.sum(s, axis=-1) + w_inter * jnp.einsum('bhd,bhtd->bht', n, qj)
        h = num / jnp.maximum(jnp.abs(den), jnp.exp(-m_t))[..., None]
        bL = b[..., -1]
        g = bL[..., None] - b + ij
        m_new = jnp.maximum(bL + m, jnp.max(g, axis=-1))
        decay = jnp.exp(bL + m - m_new)
        wk = jnp.exp(g - m_new[..., None])
        C = decay[..., None, None] * C + jnp.einsum('bhsv,bhsd->bhvd', vj * wk[..., None], kj)
        n = decay[..., None] * n + jnp.einsum('bhs,bhsd->bhd', wk, kj)
        return (C, n, m_new), h

    init = (jnp.zeros((B, H, Dv, Dk), jnp.float32), jnp.zeros((B, H, Dk), jnp.float32),
            jnp.zeros((B, H), jnp.float32))
    _, hc = lax.scan(step, init, (qc, kc, vc, ic, fc))
    return jnp.moveaxis(hc, 0, 2).reshape(B, H, S, Dv)


def mlstm_mixer(h, w_in, conv_w, conv_b, gate_b, head_gain, w_out):
    B, S, _ = h.shape
    z = h @ w_in
    qk_raw = z[..., :2 * MLSTM_QK_WIDTH]
    v = z[..., 2 * MLSTM_QK_WIDTH:2 * MLSTM_QK_WIDTH + MLSTM_V_WIDTH]
    o = z[..., 2 * MLSTM_QK_WIDTH + MLSTM_V_WIDTH:2 * MLSTM_QK_WIDTH + 2 * MLSTM_V_WIDTH]
    gates = z[..., 2 * MLSTM_QK_WIDTH + 2 * MLSTM_V_WIDTH:]
    qk = jax.nn.silu(centred_depthwise_conv(qk_raw, conv_w, conv_b))
    q = qk[..., :MLSTM_QK_WIDTH].reshape(B, S, MLSTM_HEADS, MLSTM_QK_DIM).transpose(0, 2, 1, 3)
    k = (qk[..., MLSTM_QK_WIDTH:] * MLSTM_QK_DIM ** -0.5).reshape(B, S, MLSTM_HEADS, MLSTM_QK_DIM).transpose(0, 2, 1, 3)
    v = v.reshape(B, S, MLSTM_HEADS, MLSTM_V_DIM).transpose(0, 2, 1, 3)
    g = (gates.astype(jnp.float32) + gate_b.astype(jnp.float32)).reshape(B, S, 2, 2, MLSTM_HEADS)
    g = jnp.transpose(g, (2, 3, 0, 4, 1))
    h_fwd = mlstm_chunkwise(q, k, v, g[0, 0], jax.nn.log_sigmoid(g[0, 1]))
    flip = lambda a: jnp.flip(a, axis=2)
    h_bwd = flip(mlstm_chunkwise(flip(q), flip(k), flip(v), flip(g[1, 0]), flip(jax.nn.log_sigmoid(g[1, 1]))))
    hs = (h_fwd + h_bwd).transpose(0, 2, 1, 3).astype(h.dtype)
    hs = rms_norm(hs) * head_gain
    out = jax.nn.sigmoid(o).reshape(B, S, MLSTM_HEADS, MLSTM_V_DIM) * hs
    return out.reshape(B, S, MLSTM_V_WIDTH) @ w_out


def swiglu(h, w1, w3, w2):
    return (jax.nn.silu(h @ w1) * (h @ w3)) @ w2


def setup_inputs(seed: int = 0) -> dict:
    key = jax.random.key(seed)
    ks = jax.random.split(key, 24)
    n_ev = (DEPTH + 1) // 2
    n_od = DEPTH // 2
    f32 = jnp.float32

    def nrm(k, shape, scale):
        return jax.random.normal(k, shape, f32) * scale

    i_bias = nrm(ks[14], (n_od, 2, MLSTM_HEADS), 0.1)
    f_bias = jnp.linspace(3.0, 6.0, MLSTM_HEADS, dtype=f32)[None, None, :] + nrm(ks[15], (n_od, 2, MLSTM_HEADS), 0.1)
    return {
        'x': nrm(ks[0], (BATCH, SEQ, D_MODEL), 1.0),
        'c': nrm(ks[1], (BATCH, D_MODEL), 1.0),
        'ada_w': nrm(ks[2], (DEPTH, D_MODEL, 6 * D_MODEL), D_MODEL ** -0.5),
        'ada_b': nrm(ks[3], (DEPTH, 6 * D_MODEL), 0.02),
        'ev_w_in': nrm(ks[4], (n_ev, D_MODEL, EVEN_IN_DIM), D_MODEL ** -0.5),
        'ev_w_pool': nrm(ks[5], (n_ev, N_POOL_GROUPS, POOL_GROUP_DIM, POOL_GROUP_DIM), POOL_GROUP_DIM ** -0.5),
        'ev_b_pool': nrm(ks[6], (n_ev, N_POOL_GROUPS, POOL_GROUP_DIM), 0.02),
        'ev_pool_scale': 1.0 + nrm(ks[7], (n_ev, POOL_DIM), 0.1),
        'ev_q_gain': 1.0 + nrm(ks[8], (n_ev, ATT_HEAD_DIM), 0.1),
        'ev_k_gain': 1.0 + nrm(ks[9], (n_ev, ATT_HEAD_DIM), 0.1),
        'ev_w_out': nrm(ks[10], (n_ev, EVEN_MIX_DIM, D_MODEL), EVEN_MIX_DIM ** -0.5),
        'od_w_in': nrm(ks[11], (n_od, D_MODEL, ODD_IN_DIM), D_MODEL ** -0.5),
        'od_conv_w': nrm(ks[12], (n_od, CONV_K, 2 * MLSTM_QK_WIDTH), CONV_K ** -0.5),
        'od_conv_b': nrm(ks[13], (n_od, 2 * MLSTM_QK_WIDTH), 0.02),
        'od_gate_b': jnp.stack([i_bias, f_bias], axis=2).reshape(n_od, N_GATES),
        'od_head_gain': 1.0 + nrm(ks[16], (n_od, MLSTM_HEADS, MLSTM_V_DIM), 0.1),
        'od_w_out': nrm(ks[17], (n_od, MLSTM_V_WIDTH, D_MODEL), MLSTM_V_WIDTH ** -0.5),
        'ffn_w1': nrm(ks[18], (DEPTH, D_MODEL, FFN_HIDDEN), D_MODEL ** -0.5),
        'ffn_w3': nrm(ks[19], (DEPTH, D_MODEL, FFN_HIDDEN), D_MODEL ** -0.5),
        'ffn_w2': nrm(ks[20], (DEPTH, FFN_HIDDEN, D_MODEL), FFN_HIDDEN ** -0.5),
    }


def reference(x, c, ada_w, ada_b, ev_w_in, ev_w_pool, ev_b_pool, ev_pool_scale, ev_q_gain, ev_k_gain,
              ev_w_out, od_w_in, od_conv_w, od_conv_b, od_gate_b, od_head_gain, od_w_out,
              ffn_w1, ffn_w3, ffn_w2):
    cos, sin = grid_rope(x.shape[1])
    cond = jax.nn.silu(c)
    for layer in range(DEPTH):
        mod = cond @ ada_w[layer] + ada_b[layer]
        sh1, sc1, g1, sh2, sc2, g2 = jnp.split(mod, 6, axis=-1)
        h = modulate(rms_norm(x), sh1, sc1)
        j = layer // 2
        if layer % 2 == 0:
            y = pool_attention_mixer(h, ev_w_in[j], ev_w_pool[j], ev_b_pool[j], ev_pool_scale[j],
                                     ev_q_gain[j], ev_k_gain[j], ev_w_out[j], cos, sin)
        else:
            y = mlstm_mixer(h, od_w_in[j], od_conv_w[j], od_conv_b[j], od_gate_b[j],
                            od_head_gain[j], od_w_out[j])
        x = x + g1[:, None, :] * y
        h = modulate(rms_norm(x), sh2, sc2)
        x = x + g2[:, None, :] * swiglu(h, ffn_w1[layer], ffn_w3[layer], ffn_w2[layer])
    return x
```

```python
import contextlib
import numpy as np
import concourse.bass as bass
import concourse.mybir as mybir
from concourse.bass_utils import run_bass_kernel_spmd

F32 = mybir.dt.float32
BF16 = mybir.dt.bfloat16
AF = mybir.ActivationFunctionType
ALU = mybir.AluOpType
AX = mybir.AxisListType

NCORES = 8
BATCH = 32
SEQ = 2048
DM = 1024
KC = 8
FH = 2816
HC = 22
NL = 4
EPS = 1e-6
NT = SEQ // 128
NB = SEQ // 512


class Stream:
    __slots__ = ("sem", "name", "val")

    def __init__(self, sem, name):
        self.sem = sem
        self.name = name
        self.val = 0


class Buf:
    __slots__ = ("name", "w", "r")

    def __init__(self, name=""):
        self.name = name
        self.w = None
        self.r = {}


ENGS = ("tensor", "vector", "scalar", "gpsimd", "sync")


class Sched:
    def __init__(self, nc, stack, n_dma=16, same_engine_sync=True):
        self.nc = nc
        self.q = {e: [] for e in ENGS}
        self.st = {e: Stream(stack.enter_context(nc.semaphore("s_" + e)), e) for e in ENGS}
        self.dma_pool = {}
        for q_ in ("sync", "gpsimd", "scalar"):
            self.dma_pool[q_] = [Stream(stack.enter_context(nc.semaphore("d%s%d" % (q_, i))), "d%s%d" % (q_, i)) for i in range(n_dma if q_ != "scalar" else 2)]
        self.dma_st = [s_ for q_ in self.dma_pool for s_ in self.dma_pool[q_]]
        self.dma_rr = {q_: 0 for q_ in self.dma_pool}
        self.seen = {e: {} for e in ENGS}
        self.same = same_engine_sync
        self.nins = {e: 0 for e in ENGS}

    def _need(self, eng, deps):
        seen = self.seen[eng]
        for (s, v) in deps:
            if s is self.st[eng]:
                if eng == "tensor" or eng == "sync" or not self.same:
                    continue
            if seen.get(s, 0) >= v:
                continue
            seen[s] = v
            self.q[eng].append(("w", s.sem, v))

    @staticmethod
    def _deps(reads, writes):
        deps = []
        for b in reads:
            if b.w is not None:
                deps.append(b.w)
        for b in writes:
            if b.w is not None:
                deps.append(b.w)
            for s, v in b.r.items():
                deps.append((s, v))
        return deps

    @staticmethod
    def _mark(tok, reads, writes):
        s, v = tok
        for b in reads:
            if b.r.get(s, 0) < v:
                b.r[s] = v
        for b in writes:
            b.w = tok
            b.r = {}

    def op(self, eng, fn, reads=(), writes=()):
        self._need(eng, self._deps(reads, writes))
        s = self.st[eng]
        s.val += 1
        self.q[eng].append(("i", fn, s.sem, 1))
        self.nins[eng] += 1
        self._mark((s, s.val), reads, writes)

    def dma(self, eng, out, in_, reads=(), writes=()):
        pool = self.dma_pool[eng]
        d = pool[self.dma_rr[eng]]
        self.dma_rr[eng] = (self.dma_rr[eng] + 1) % len(pool)
        deps = self._deps(reads, writes)
        if d.val:
            deps.append((d, d.val))
        self._need(eng, deps)
        d.val += 16
        self.q[eng].append(("i", lambda e, o=out, i=in_: e.dma_start(out=o, in_=i), d.sem, 16))
        self.nins[eng] += 1
        self._mark((d, d.val), reads, writes)

    def barrier(self):
        deps = [(s, s.val) for s in self.dma_st if s.val] + [(self.st[e], self.st[e].val) for e in ENGS if self.st[e].val]
        for e in ENGS:
            self._need(e, deps)

    def finish(self):
        deps = [(s, s.val) for s in self.dma_st if s.val] + [(self.st[e], self.st[e].val) for e in ENGS if e != "sync" and self.st[e].val]
        self._need("sync", deps)

    def emit(self):
        nc = self.nc
        with nc.Block() as block:
            for e in ENGS:
                items = self.q[e]

                def body(engobj, items=items):
                    for it in items:
                        if it[0] == "w":
                            engobj.wait_ge(it[1], it[2])
                        else:
                            it[1](engobj).then_inc(it[2], it[3])
                getattr(block, e)(body)


POOL_W = (2, 4, 8, 16)


def _band_blocks():
    out = np.zeros((4, 5, 128, 128), np.float32)
    for g, w in enumerate(POOL_W):
        P = np.zeros((SEQ, SEQ), np.float64)
        def row(t):
            lo = min(max(t - w // 2, 0), SEQ)
            hi = min(max(t + w - w // 2, 0), SEQ)
            r = np.zeros(SEQ)
            r[lo:hi] = 1.0 / (hi - lo)
            r[t] -= 1.0
            return r
        def blk(ti, si):
            b = np.zeros((128, 128))
            for tt in range(128):
                r = row(ti * 128 + tt)
                b[:, tt] = r[si * 128:(si + 1) * 128]
            return b
        out[g, 0] = blk(5, 4)
        out[g, 1] = blk(5, 5)
        out[g, 2] = blk(5, 6)
        out[g, 3] = blk(0, 0)
        out[g, 4] = blk(NT - 1, NT - 1)
    return out


def _rope_tables():
    t = np.arange(SEQ)
    row = (t // 64).astype(np.float32)
    col = (t % 64).astype(np.float32)
    n_freq = 16
    inv_freq = (np.float32(10000.0) ** (-np.arange(n_freq, dtype=np.float32) / n_freq)).astype(np.float32)
    ang = np.concatenate([row[:, None] * inv_freq, col[:, None] * inv_freq], axis=-1).astype(np.float32)
    cos = np.cos(ang).astype(np.float32)
    sin = np.sin(ang).astype(np.float32)
    cos = cos.reshape(NT, 128, 32).transpose(1, 0, 2)
    sin = sin.reshape(NT, 128, 32).transpose(1, 0, 2)
    return np.ascontiguousarray(cos), np.ascontiguousarray(sin)


CF = {}
_off = 0
for _name, _n in (("cos", NT * 32), ("sin", NT * 32), ("tri_f", 128), ("tri_b", 128), ("ones", 128), ("ident", 128)):
    CF[_name] = (_off, _n)
    _off += _n
NCF = _off
CB = {}
_off = 0
for _name, _n in (("ident", 128), ("ones", 128), ("band", 4 * 5 * 128), ("neg_f", 128), ("neg_b", 128), ("selb", 16), ("selc", 16), ("selh", 8 * 128)):
    CB[_name] = (_off, _n)
    _off += _n
NCB = _off


def _const_tables():
    cf = np.zeros((128, NCF), np.float32)
    cos, sin = _rope_tables()
    cf[:, CF["cos"][0]:CF["cos"][0] + NT * 32] = cos.reshape(128, -1)
    cf[:, CF["sin"][0]:CF["sin"][0] + NT * 32] = sin.reshape(128, -1)
    k = np.arange(128)[:, None]
    j = np.arange(128)[None, :]
    cf[:, CF["tri_f"][0]:CF["tri_f"][0] + 128] = (k <= j)
    cf[:, CF["tri_b"][0]:CF["tri_b"][0] + 128] = (k >= j)
    cf[:, CF["ones"][0]:CF["ones"][0] + 128] = 1.0
    cf[:, CF["ident"][0]:CF["ident"][0] + 128] = np.eye(128)
    cb = np.zeros((128, NCB), np.float32)
    cb[:, CB["ident"][0]:CB["ident"][0] + 128] = np.eye(128)
    cb[:, CB["ones"][0]:CB["ones"][0] + 128] = 1.0
    bb = _band_blocks()
    cb[:, CB["band"][0]:CB["band"][0] + 4 * 5 * 128] = bb.transpose(2, 0, 1, 3).reshape(128, -1)
    cb[:, CB["neg_f"][0]:CB["neg_f"][0] + 128] = np.where(k <= j, 0.0, -30000.0)
    cb[:, CB["neg_b"][0]:CB["neg_b"][0] + 128] = np.where(k >= j, 0.0, -30000.0)
    kk = np.arange(128)[:, None]
    rr = np.arange(16)[None, :]
    cb[:, CB["selb"][0]:CB["selb"][0] + 16] = ((kk == rr) | (kk == 16 + rr))
    cb[:, CB["selc"][0]:CB["selc"][0] + 16] = ((kk == 32 + rr) | (kk == 48 + rr))
    for d_ in range(2):
        for hp_ in range(4):
            r0 = d_ * 8 + 2 * hp_
            o_ = CB["selh"][0] + (d_ * 4 + hp_) * 128
            kcol = np.arange(128)[:, 0] if False else np.arange(128)
            cb[:, o_:o_ + 64] = ((kcol == r0) | (kcol == 16 + r0))[:, None]
            cb[:, o_ + 64:o_ + 128] = ((kcol == r0 + 1) | (kcol == 17 + r0))[:, None]
    return cf, cb


def prep_shared(inp):
    f = lambda a: np.ascontiguousarray(a, dtype=np.float32)
    d = {}
    aw = inp["ada_w"].reshape(NL, KC, 128, 8, 768)
    d["adaW"] = f(aw.transpose(0, 3, 2, 1, 4).reshape(NL, 8, 128, KC * 768))
    d["adaB"] = f(inp["ada_b"].reshape(NL, 48, 128).transpose(2, 0, 1))
    w1 = inp["ffn_w1"].reshape(NL, KC, 128, 11, 2, 128)
    w3 = inp["ffn_w3"].reshape(NL, KC, 128, 11, 2, 128)
    w13 = np.stack([w1, w3], axis=0)
    d["w13"] = f(w13.transpose(1, 4, 3, 0, 5, 2, 6).reshape(NL, 11, 128, 4096))
    w2 = inp["ffn_w2"].reshape(NL, HC, 128, 4, 2, 128)
    d["w2"] = f(w2.transpose(0, 3, 2, 4, 1, 5).reshape(NL, 4, 128, 2 * HC * 128))
    d["evWin"] = f(inp["ev_w_in"].reshape(2, KC, 128, 1280).transpose(0, 2, 1, 3).reshape(2, 128, KC * 1280))
    d["evWpool"] = f(inp["ev_w_pool"].transpose(0, 2, 1, 3).reshape(2, 128, 512))
    d["evBpool"] = f(inp["ev_b_pool"].transpose(2, 0, 1))
    d["evPscale"] = f(inp["ev_pool_scale"].reshape(2, 4, 128).transpose(2, 0, 1))
    d["evGain"] = f(np.concatenate([np.tile(inp["ev_q_gain"], (1, 8)), np.tile(inp["ev_k_gain"], (1, 2))], axis=1))
    wo = inp["ev_w_out"]
    d["evWoutP"] = f(wo[:, :512].reshape(2, 4, 128, DM).transpose(0, 2, 1, 3).reshape(2, 128, 4 * DM))
    d["evWoutA"] = f(wo[:, 512:].reshape(2, 8, 64, DM).transpose(0, 2, 1, 3).reshape(2, 64, 8 * DM))
    wi = inp["od_w_in"]
    per_hp = []
    for hp in range(4):
        cols = np.concatenate([np.arange(hp * 128, hp * 128 + 128), 512 + np.arange(hp * 128, hp * 128 + 128),
                               1024 + np.arange(hp * 256, hp * 256 + 256), 2048 + np.arange(hp * 256, hp * 256 + 256)])
        per_hp.append(wi[:, :, cols])
    whp = np.stack(per_hp, axis=1).reshape(2, 4, KC, 128, 768)
    d["odWin"] = f(whp.transpose(0, 1, 3, 2, 4).reshape(2, 4, 128, KC * 768))
    d["odWg"] = f(wi[:, :, 3072:3104].reshape(2, KC, 128, 32).transpose(0, 2, 1, 3).reshape(2, 128, KC * 32))
    d["odConvW"] = f(inp["od_conv_w"].reshape(2, 5, 8, 128).transpose(3, 0, 2, 1))
    d["odConvB"] = f(inp["od_conv_b"].reshape(2, 8, 128).transpose(2, 0, 1))
    d["odGateB"] = f(inp["od_gate_b"])
    d["odHGain"] = f(inp["od_head_gain"].reshape(2, 1024))
    d["odWout"] = f(inp["od_w_out"].reshape(2, 4, 2, 128, DM).transpose(0, 1, 3, 2, 4).reshape(2, 4, 128, 2 * DM))
    cf, cb = _const_tables()
    d["cstF"] = cf
    d["cstB"] = cb
    return d


def prep_core(inp, bidx):
    x = inp["x"][bidx]
    ns = len(bidx)
    xT = np.ascontiguousarray(x.reshape(ns, SEQ, KC, 128).transpose(0, 3, 2, 1), dtype=np.float32)
    c = inp["c"][bidx]
    cT = np.ascontiguousarray(c.reshape(ns, KC, 128).transpose(2, 1, 0).reshape(128, KC * ns), dtype=np.float32)
    return {"xT": xT, "cT": cT}


SHARED_SHAPES = {
    "adaW": [NL, 8, 128, KC * 768], "adaB": [128, NL, 48], "w13": [NL, 11, 128, 4096], "w2": [NL, 4, 128, 2 * HC * 128],
    "evWin": [2, 128, KC * 1280], "evWpool": [2, 128, 512], "evBpool": [128, 2, 4], "evPscale": [128, 2, 4],
    "evGain": [2, 640], "evWoutP": [2, 128, 4 * DM], "evWoutA": [2, 64, 8 * DM],
    "odWin": [2, 4, 128, KC * 768], "odWg": [2, 128, KC * 32], "odConvW": [128, 2, 8, 5], "odConvB": [128, 2, 8],
    "odGateB": [2, 32], "odHGain": [2, 1024], "odWout": [2, 4, 128, 2 * DM],
    "cstF": [128, NCF], "cstB": [128, NCB],
}


DBG = {}
WQ = "gpsimd"
ARENA_BYTES = 58 * 1024
WREG_BYTES = 34560


class Prog:
    def __init__(self, ns, plan, first_layer_mod=True):
        self.ns = ns
        self.plan = plan
        nc = self.nc = bass.Bass("TRN2", target_bir_lowering=False)
        self.dr = {}
        self.dr["xT"] = nc.dram_tensor("xT", [ns, 128, KC, SEQ], F32, kind="ExternalInput").ap()
        self.dr["cT"] = nc.dram_tensor("cT", [128, KC * ns], F32, kind="ExternalInput").ap()
        for k, sh in SHARED_SHAPES.items():
            self.dr[k] = nc.dram_tensor(k, sh, F32, kind="ExternalInput").ap()
        self.dr["yT"] = nc.dram_tensor("yT", [ns, 128, KC, SEQ], F32, kind="ExternalOutput").ap()
        with contextlib.ExitStack() as st:
            self.stack = st
            self.S = Sched(nc, st)
            self._alloc()
            self._emit_all()
            self.S.finish()
            self.S.emit()

    def sb(self, name, shape, dt):
        return self.stack.enter_context(self.nc.sbuf_tensor(name, shape, dt))

    def view(self, region, boff, shape, dt):
        esz = 4 if dt == F32 else 2
        n = int(np.prod(shape))
        assert boff % 4 == 0
        a = region[:, boff // 2: boff // 2 + n * esz // 2]
        if dt == F32:
            a = a.bitcast(F32)
        if len(shape) == 2:
            a = a.rearrange("p (a b) -> p a b", a=shape[0])
        elif len(shape) == 3:
            a = a.rearrange("p (a b c) -> p a b c", a=shape[0], b=shape[1])
        return a

    def _alloc(self):
        nc = self.nc
        ns = self.ns
        self.XT = self.sb("XT", [128, KC, SEQ], F32)
        self.HTr = self.sb("HTr", [128, KC * SEQ], BF16)
        self.HT = self.HTr[:, :].rearrange("p (a b) -> p a b", a=KC)
        self.AR = self.sb("AR", [128, ARENA_BYTES // 2], BF16)
        self.WR = self.sb("WR", [128, WREG_BYTES // 2], BF16)
        self.CSTF = self.sb("CSTF", [128, NCF], F32)
        self.CSTB = self.sb("CSTB", [128, NCB], BF16)
        self.MOD = self.sb("MOD", [128, NL, 48, ns], F32)
        self.MODP1 = self.sb("MODP1", [128, NL, 2, 8, ns], F32)
        self.ADAB = self.sb("ADAB", [128, NL, 48], F32)
        self.COND = self.sb("COND", [128, KC * ns], F32)
        self.EVB = self.sb("EVB", [128, 2, 4], F32)
        self.EVS = self.sb("EVS", [128, 2, 4], F32)
        self.SM = self.sb("SM", [128, 64], F32)
        self.CONVW = self.sb("CONVW", [128, 2, 8, 5], F32)
        self.CONVB = self.sb("CONVB", [128, 2, 8], F32)
        self.PS = [self.stack.enter_context(nc.psum_tensor("ps%d" % i, [128, 512], F32)) for i in range(8)]
        self.PSB = [Buf("ps%d" % i) for i in range(8)]
        self.bXT = [[Buf("xt%d_%d" % (c, tb)) for tb in range(NB)] for c in range(KC)]
        self.bHT = [Buf("ht%d" % tb) for tb in range(NB)]
        self.bCST = Buf("cst")
        self.bMOD = Buf("mod")

    def cf(self, name):
        o, n = CF[name]
        return self.CSTF[:, o:o + n]

    def cb(self, name):
        o, n = CB[name]
        return self.CSTB[:, o:o + n]

    def mm(self, out, lhsT, rhs, start, stop, reads, writes):
        self.S.op("tensor", lambda e: e.matmul(out, lhsT=lhsT, rhs=rhs, start=start, stop=stop), reads=reads, writes=writes)

    def tr(self, out, in_, ident, reads, writes):
        self.S.op("tensor", lambda e: e.transpose(out=out, in_=in_, identity=ident), reads=reads, writes=writes)

    def act(self, out, in_, func, reads, writes, **kw):
        self.S.op("scalar", lambda e: e.activation(out=out, in_=in_, func=func, **kw), reads=reads, writes=writes)

    def tt(self, eng, out, in0, in1, op, reads, writes):
        self.S.op(eng, lambda e: e.tensor_tensor(out=out, in0=in0, in1=in1, op=op), reads=reads, writes=writes)

    def ts(self, eng, out, in0, s1, s2, op0, op1, reads, writes):
        if op1 is None:
            self.S.op(eng, lambda e: e.tensor_scalar(out=out, in0=in0, scalar1=s1, scalar2=None, op0=op0), reads=reads, writes=writes)
        else:
            self.S.op(eng, lambda e: e.tensor_scalar(out=out, in0=in0, scalar1=s1, scalar2=s2, op0=op0, op1=op1), reads=reads, writes=writes)

    def stt(self, out, in0, scalar, in1, op0, op1, reads, writes):
        self.S.op("vector", lambda e: e.scalar_tensor_tensor(out=out, in0=in0, scalar=scalar, in1=in1, op0=op0, op1=op1), reads=reads, writes=writes)

    def cp(self, eng, out, in_, reads, writes):
        self.S.op(eng, lambda e: e.tensor_copy(out=out, in_=in_), reads=reads, writes=writes)

    def ms(self, eng, ap, val, writes):
        self.S.op(eng, lambda e: e.memset(ap, val), writes=writes)

    def rcp(self, out, in_, reads, writes):
        self.S.op("vector", lambda e: e.reciprocal(out=out, in_=in_), reads=reads, writes=writes)

    def _emit_all(self):
        S = self.S
        S.dma("sync", self.CSTF[:, :], self.dr["cstF"], writes=[self.bCST])
        S.dma(WQ, self.CSTB[:, :], self.dr["cstB"], writes=[self.bCST])
        S.dma("sync", self.EVB[:, :, :], self.dr["evBpool"], writes=[self.bCST])
        S.dma("sync", self.EVS[:, :, :], self.dr["evPscale"], writes=[self.bCST])
        S.dma("sync", self.CONVW[:, :, :, :], self.dr["odConvW"], writes=[self.bCST])
        S.dma("sync", self.CONVB[:, :, :], self.dr["odConvB"], writes=[self.bCST])
        self.emit_ada()
        S.barrier()
        for n in range(self.ns):
            for c in range(KC):
                S.dma("sync", self.XT[:, c, :], self.dr["xT"][n, :, c, :], writes=self.bXT[c])
            for (kind, l) in self.plan:
                if kind == "ffn":
                    self.emit_ffn(l, n)
                elif kind == "mix" and l % 2 == 0:
                    self.emit_even(l, n)
                elif kind == "mix":
                    self.emit_odd(l, n)
                S.barrier()
            for c in range(KC):
                S.dma("sync", self.dr["yT"][n, :, c, :], self.XT[:, c, :], reads=self.bXT[c])

    def emit_ada(self):
        S, ns = self.S, self.ns
        S.dma("sync", self.COND[:, :], self.dr["cT"], writes=[self.bMOD])
        S.dma("sync", self.ADAB[:, :, :], self.dr["adaB"], writes=[self.bMOD])
        S.op("scalar", lambda e: e.activation(out=self.COND[:, :], in_=self.COND[:, :], func=AF.Silu), reads=[self.bMOD], writes=[self.bMOD])
        AW = [self.view(self.AR, i * 24576, [KC * 768], F32) for i in range(2)]
        bAW = [Buf("aw0"), Buf("aw1")]
        ps = self.PS[0]
        pb = self.PSB[0]
        k = 0
        for l in range(NL):
            for q in range(8):
                w, bw = AW[k % 2], bAW[k % 2]
                k += 1
                S.dma("sync", w, self.dr["adaW"][l, q], writes=[bw])
                for fcl in range(6):
                    fc = q * 6 + fcl
                    for kc in range(KC):
                        S.op("tensor", lambda e, w=w, fcl=fcl, kc=kc, fc=fc: e.matmul(
                            ps[:, fc * ns:(fc + 1) * ns], lhsT=w[:, kc * 768 + fcl * 128: kc * 768 + fcl * 128 + 128],
                            rhs=self.COND[:, kc * ns:(kc + 1) * ns], start=(kc == 0), stop=(kc == KC - 1)),
                            reads=[bw, self.bMOD], writes=[pb])
            S.op("vector", lambda e, l=l: e.tensor_tensor(
                out=self.MOD[:, l, :, :], in0=ps[:, 0:48 * ns].rearrange("p (a b) -> p a b", a=48),
                in1=self.ADAB[:, l, :].unsqueeze(2).to_broadcast([128, 48, ns]), op=ALU.add),
                reads=[pb, self.bMOD], writes=[self.bMOD])
            for i, base in enumerate((8, 32)):
                S.op("vector", lambda e, l=l, i=i, base=base: e.tensor_scalar(
                    out=self.MODP1[:, l, i, :, :], in0=self.MOD[:, l, base:base + 8, :], scalar1=1.0, scalar2=None, op0=ALU.add),
                    reads=[self.bMOD], writes=[self.bMOD])

    def emit_norm(self, l, which, n, scr_off):
        S = self.S
        SQ = [self.view(self.AR, scr_off + i * 1024, [512], BF16) for i in range(2)]
        RS = self.view(self.AR, scr_off + 2048, [512], F32)
        TMP = [self.view(self.AR, scr_off + 4096 + i * 2048, [512], F32) for i in range(2)]
        bSQ = [Buf(), Buf()]
        bRS = Buf()
        bTMP = [Buf(), Buf()]
        ps, pb = self.PS[7], self.PSB[7]
        ones = self.cb("ones")
        shb = 0 if which == 0 else 24
        for tb in range(NB):
            ts = slice(tb * 512, (tb + 1) * 512)
            for c in range(KC):
                S.op("scalar", lambda e, c=c, ts=ts: e.activation(out=SQ[c % 2], in_=self.XT[:, c, ts], func=AF.Square),
                     reads=[self.bXT[c][tb]], writes=[bSQ[c % 2]])
                S.op("tensor", lambda e, c=c: e.matmul(ps[:, :], lhsT=ones, rhs=SQ[c % 2], start=(c == 0), stop=(c == KC - 1)),
                     reads=[bSQ[c % 2], self.bCST], writes=[pb])
            S.op("vector", lambda e: e.tensor_scalar(out=RS, in0=ps[:, :], scalar1=1.0 / DM, scalar2=EPS, op0=ALU.mult, op1=ALU.add),
                 reads=[pb], writes=[bRS])
            S.op("scalar", lambda e: e.activation(out=RS, in_=RS, func=AF.Sqrt), reads=[bRS], writes=[bRS])
            S.op("vector", lambda e: e.reciprocal(out=RS, in_=RS), reads=[bRS], writes=[bRS])
            for c in range(KC):
                S.op("vector", lambda e, c=c, ts=ts: e.tensor_tensor(out=TMP[c % 2], in0=self.XT[:, c, ts], in1=RS, op=ALU.mult),
                     reads=[self.bXT[c][tb], bRS], writes=[bTMP[c % 2]])
                S.op("scalar", lambda e, c=c, ts=ts: e.activation(
                    out=self.HT[:, c, ts], in_=TMP[c % 2], func=AF.Identity,
                    bias=self.MOD[:, l, shb + c, n:n + 1], scale=self.MODP1[:, l, which, c, n:n + 1]),
                    reads=[bTMP[c % 2], self.bMOD], writes=[self.bHT[tb]])

    def emit_ffn(self, l, n):
        S = self.S
        self.emit_norm(l, 1, n, 48 * 1024)
        AT = self.view(self.AR, 0, [HC, 1024], BF16)
        SG = [self.view(self.AR, 44 * 1024 + i * 2048, [512], F32) for i in range(2)]
        bAT = [[Buf() for _ in range(2)] for _ in range(HC)]
        bSG = [Buf(), Buf()]
        WS = [self.WR[:, i * 5632:(i + 1) * 5632] for i in range(3)]
        bWS = [Buf(), Buf(), Buf()]
        wk = 0
        pk = 0
        for half in range(2):
            for jp in range(11):
                ws, bw = WS[wk % 3], bWS[wk % 3]
                wk += 1
                S.dma(WQ, ws[:, 0:4096], self.dr["w13"][l, jp], writes=[bw])
                for jj in range(2):
                    j = 2 * jp + jj
                    for tb2 in range(2):
                        tb = half * 2 + tb2
                        ts = slice(tb * 512, (tb + 1) * 512)
                        pg, pgb = self.PS[pk % 2], self.PSB[pk % 2]
                        pu, pub = self.PS[2 + pk % 2], self.PSB[2 + pk % 2]
                        sg, bsg = SG[pk % 2], bSG[pk % 2]
                        pk += 1
                        for (pp, ppb, wsel) in ((pg, pgb, 0), (pu, pub, 1)):
                            for kc in range(KC):
                                o = (wsel * 2 + jj) * 1024 + kc * 128
                                S.op("tensor", lambda e, pp=pp, o=o, kc=kc, ts=ts, ws=ws: e.matmul(
                                    pp[:, :], lhsT=ws[:, o:o + 128], rhs=self.HT[:, kc, ts], start=(kc == 0), stop=(kc == KC - 1)),
                                    reads=[bw, self.bHT[tb]], writes=[ppb])
                        S.op("scalar", lambda e, pg=pg, sg=sg: e.activation(out=sg, in_=pg[:, :], func=AF.Silu), reads=[pgb], writes=[bsg])
                        S.op("vector", lambda e, pu=pu, sg=sg, j=j, tb2=tb2: e.tensor_tensor(
                            out=AT[:, j, tb2 * 512:(tb2 + 1) * 512], in0=pu[:, :], in1=sg, op=ALU.mult),
                            reads=[pub, bsg], writes=[bAT[j][tb2]])
            for cp in range(4):
                ws, bw = WS[wk % 3], bWS[wk % 3]
                wk += 1
                S.dma(WQ, ws[:, 0:5632], self.dr["w2"][l, cp], writes=[bw])
                for cc in range(2):
                    c = 2 * cp + cc
                    for tb2 in range(2):
                        tb = half * 2 + tb2
                        ts = slice(tb * 512, (tb + 1) * 512)
                        po, pob = self.PS[4 + pk % 2], self.PSB[4 + pk % 2]
                        pk += 1
                        for j in range(HC):
                            o = cc * HC * 128 + j * 128
                            S.op("tensor", lambda e, po=po, o=o, j=j, tb2=tb2, ws=ws: e.matmul(
                                po[:, :], lhsT=ws[:, o:o + 128], rhs=AT[:, j, tb2 * 512:(tb2 + 1) * 512], start=(j == 0), stop=(j == HC - 1)),
                                reads=[bw, bAT[j][tb2]], writes=[pob])
                        S.op("vector", lambda e, po=po, c=c, ts=ts: e.scalar_tensor_tensor(
                            out=self.XT[:, c, ts], in0=po[:, :], scalar=self.MOD[:, l, 40 + c, n:n + 1], in1=self.XT[:, c, ts],
                            op0=ALU.mult, op1=ALU.add),
                            reads=[pob, self.bMOD, self.bXT[c][tb]], writes=[self.bXT[c][tb]])

    def emit_even(self, l, n):
        S = self.S
        j = l // 2
        K1 = 1024
        self.emit_norm(l, 0, n, 48 * K1)
        S.barrier()
        U = self.view(self.AR, 0, [NT, 512], BF16)
        QT = self.view(self.AR, 16 * K1, [4, SEQ], BF16)
        KZ = self.view(self.AR, 32 * K1, [4, SEQ], BF16)
        V = self.view(self.AR, 48 * K1, [NT, 2, 128], BF16)
        T1 = self.view(self.WR, 20480, [320], F32)
        T2 = self.view(self.WR, 20480 + 1280, [320], F32)
        QROT = self.view(self.WR, 20480 + 2560, [640], BF16)
        SQ = self.view(self.WR, 28160, [640], F32)
        QN = self.view(self.WR, 28160 + 2560, [640], F32)
        KD = self.view(self.WR, 33280, [512], BF16)
        Win = self.WR[:, 0:KC * 1280]
        Wpool = self.WR[:, 12288:12288 + 512]
        GAIN = self.WR[:, 12800:12800 + 1280].bitcast(F32)
        bWin, bWp, bGain = Buf(), Buf(), Buf()
        bU = [Buf() for _ in range(NT)]
        bQK = [Buf() for _ in range(NT)]
        bV = [Buf() for _ in range(NT)]
        bVones = Buf()
        bSQ, bQN, bT1, bT2, bQROT, bKD, bSM = (Buf() for _ in range(7))
        for hh in range(2):
            S.dma(WQ, Win[:, hh * 5120:(hh + 1) * 5120], self.dr["evWin"][j, :, hh * 5120:(hh + 1) * 5120], writes=[bWin])
        S.dma(WQ, Wpool, self.dr["evWpool"][j], writes=[bWp])
        S.dma("sync", GAIN, self.dr["evGain"][j].partition_broadcast(128), writes=[bGain])
        S.op("gpsimd", lambda e: e.memset(V[:, :, :, 64:128], 1.0), writes=[bVones])
        S.op("gpsimd", lambda e: e.memset(KD, 0.0), writes=[bKD])
        ident = self.cb("ident")
        SSQ = self.SM[:, 0:10]
        RST = self.SM[:, 16:26]
        cosv = self.cf("cos").rearrange("p (a b) -> p a b", a=NT)
        sinv = self.cf("sin").rearrange("p (a b) -> p a b", a=NT)
        for tt in range(NT):
            tsl = slice(tt * 128, (tt + 1) * 128)
            tb = tt // 4
            pu, pub = self.PS[tt % 2], self.PSB[tt % 2]
            pq, pqb = self.PS[2 + tt % 2], self.PSB[2 + tt % 2]
            pkv, pkvb = self.PS[4 + tt % 2], self.PSB[4 + tt % 2]
            for (pp, ppb, c0, cn) in ((pu, pub, 0, 512), (pq, pqb, 512, 512), (pkv, pkvb, 1024, 256)):
                for kc in range(KC):
                    S.op("tensor", lambda e, pp=pp, kc=kc, tsl=tsl, c0=c0, cn=cn: e.matmul(
                        pp[:, 0:cn], lhsT=self.HT[:, kc, tsl], rhs=Win[:, kc * 1280 + c0: kc * 1280 + c0 + cn],
                        start=(kc == 0), stop=(kc == KC - 1)), reads=[bWin, self.bHT[tb]], writes=[ppb])
            S.op("scalar", lambda e, pu=pu, tt=tt: e.activation(out=U[:, tt, :], in_=pu[:, :], func=AF.Copy), reads=[pub], writes=[bU[tt]])
            S.op("scalar", lambda e, pkv=pkv, tt=tt: e.activation(
                out=V[:, tt, :, 0:64], in_=pkv[:, 128:256].rearrange("p (a b) -> p a b", a=2), func=AF.Copy),
                reads=[pkvb], writes=[bV[tt]])
            S.op("scalar", lambda e, pq=pq: e.activation(out=SQ[:, 0:512], in_=pq[:, :], func=AF.Square), reads=[pqb], writes=[bSQ])
            S.op("scalar", lambda e, pkv=pkv: e.activation(out=SQ[:, 512:640], in_=pkv[:, 0:128], func=AF.Square), reads=[pkvb], writes=[bSQ])
            S.op("vector", lambda e: e.tensor_reduce(out=SSQ, in_=SQ.rearrange("p (a b) -> p a b", a=10), axis=AX.X, op=ALU.add),
                 reads=[bSQ], writes=[bSM])
            S.op("vector", lambda e: e.tensor_scalar(out=RST, in0=SSQ, scalar1=1.0 / 64, scalar2=EPS, op0=ALU.mult, op1=ALU.add),
                 reads=[bSM], writes=[bSM])
            S.op("scalar", lambda e: e.activation(out=RST, in_=RST, func=AF.Sqrt), reads=[bSM], writes=[bSM])
            S.op("vector", lambda e: e.reciprocal(out=RST, in_=RST), reads=[bSM], writes=[bSM])
            S.op("vector", lambda e, pq=pq: e.tensor_tensor(
                out=QN[:, 0:512].rearrange("p (a b) -> p a b", a=8), in0=pq[:, :].rearrange("p (a b) -> p a b", a=8),
                in1=RST[:, 0:8].unsqueeze(2).to_broadcast([128, 8, 64]), op=ALU.mult), reads=[pqb, bSM], writes=[bQN])
            S.op("vector", lambda e, pkv=pkv: e.tensor_tensor(
                out=QN[:, 512:640].rearrange("p (a b) -> p a b", a=2), in0=pkv[:, 0:128].rearrange("p (a b) -> p a b", a=2),
                in1=RST[:, 8:10].unsqueeze(2).to_broadcast([128, 2, 64]), op=ALU.mult), reads=[pkvb, bSM], writes=[bQN])
            S.op("vector", lambda e: e.tensor_tensor(out=QN, in0=QN, in1=GAIN, op=ALU.mult), reads=[bQN, bGain], writes=[bQN])
            x1 = QN[:, 0:640:2].rearrange("p (a b) -> p a b", a=10)
            x2 = QN[:, 1:640:2].rearrange("p (a b) -> p a b", a=10)
            o1 = QROT[:, 0:640:2].rearrange("p (a b) -> p a b", a=10)
            o2 = QROT[:, 1:640:2].rearrange("p (a b) -> p a b", a=10)
            t1 = T1.rearrange("p (a b) -> p a b", a=10)
            t2 = T2.rearrange("p (a b) -> p a b", a=10)
            cs = cosv[:, tt, :].unsqueeze(1).to_broadcast([128, 10, 32])
            sn = sinv[:, tt, :].unsqueeze(1).to_broadcast([128, 10, 32])
            S.op("vector", lambda e, cs=cs: e.tensor_tensor(out=t1, in0=x1, in1=cs, op=ALU.mult), reads=[bQN, self.bCST], writes=[bT1])
            S.op("gpsimd", lambda e, sn=sn: e.tensor_tensor(out=t2, in0=x2, in1=sn, op=ALU.mult), reads=[bQN, self.bCST], writes=[bT2])
            S.op("vector", lambda e: e.tensor_tensor(out=o1, in0=t1, in1=t2, op=ALU.subtract), reads=[bT1, bT2], writes=[bQROT])
            S.op("vector", lambda e, sn=sn: e.tensor_tensor(out=t1, in0=x1, in1=sn, op=ALU.mult), reads=[bQN, self.bCST], writes=[bT1])
            S.op("gpsimd", lambda e, cs=cs: e.tensor_tensor(out=t2, in0=x2, in1=cs, op=ALU.mult), reads=[bQN, self.bCST], writes=[bT2])
            S.op("vector", lambda e: e.tensor_tensor(out=o2, in0=t1, in1=t2, op=ALU.add), reads=[bT1, bT2], writes=[bQROT])
            KD3 = KD.rearrange("p (a x) -> p a x", a=2)
            krot = QROT[:, 512:640].rearrange("p (a c) -> p a c", a=2)
            S.op("gpsimd", lambda e: e.tensor_copy(out=KD3[:, :, 0:64], in_=krot), reads=[bQROT], writes=[bKD])
            S.op("gpsimd", lambda e: e.tensor_copy(out=KD3[:, :, 192:256], in_=krot), reads=[bQROT], writes=[bKD])
            ptr, ptrb = self.PS[6 + tt % 2], self.PSB[6 + tt % 2]
            ptv = ptr[:, :].bitcast(BF16)
            for hp in range(4):
                S.op("tensor", lambda e, hp=hp, ptv=ptv: e.transpose(out=ptv[:, hp * 128:(hp + 1) * 128], in_=QROT[:, hp * 128:(hp + 1) * 128], identity=ident),
                     reads=[bQROT, self.bCST], writes=[ptrb])
            for v_ in range(4):
                S.op("tensor", lambda e, v_=v_, ptv=ptv: e.transpose(out=ptv[:, 512 + v_ * 128:512 + (v_ + 1) * 128], in_=KD[:, v_ * 128:(v_ + 1) * 128], identity=ident),
                     reads=[bKD, self.bCST], writes=[ptrb])
            S.op("scalar", lambda e, ptv=ptv, tsl=tsl: e.activation(out=QT[:, :, tsl], in_=ptv[:, 0:512].rearrange("p (a b) -> p a b", a=4), func=AF.Copy),
                 reads=[ptrb], writes=[bQK[tt]])
            S.op("scalar", lambda e, ptv=ptv, tsl=tsl: e.activation(out=KZ[:, :, tsl], in_=ptv[:, 512:1024].rearrange("p (a b) -> p a b", a=4), func=AF.Copy),
                 reads=[ptrb], writes=[bQK[tt]])
        S.barrier()
        PT = [self.view(self.HTr, i * K1, [512], BF16) for i in range(3)]
        RCP = [self.view(self.HTr, 4 * K1 + i * 2 * K1, [512], F32) for i in range(2)]
        POOLED = [self.view(self.HTr, 8 * K1 + i * K1, [512], BF16) for i in range(2)]
        MIXT = [self.view(self.HTr, 10 * K1 + i * 4 * K1, [4, 512], BF16) for i in range(2)]
        ATT = self.view(self.HTr, 18 * K1, [8, 512], BF16)
        bPT = [Buf() for _ in range(3)]
        bRCP = [Buf(), Buf()]
        bPOOLED = [Buf(), Buf()]
        bMIXT = [[Buf() for _ in range(4)] for _ in range(2)]
        bATT = [Buf() for _ in range(8)]
        WoutP = self.WR[:, 0:4096]
        WoutA = self.WR[0:64, 4096:4096 + 8192]
        bWoP, bWoA = Buf(), Buf()
        S.dma(WQ, WoutP, self.dr["evWoutP"][j], writes=[bWoP])
        S.dma(WQ, WoutA, self.dr["evWoutA"][j], writes=[bWoA])
        band = self.cb("band").rearrange("p (g r t) -> p g r t", g=4, r=5)
        pk = 0
        si = 0
        for qb in range(NB):
            qs = slice(qb * 512, (qb + 1) * 512)
            mixt, bmx = MIXT[qb % 2], bMIXT[qb % 2]
            for g in range(4):
                pp, ppb = self.PS[6], self.PSB[6]
                for ti in range(4):
                    tt = qb * 4 + ti
                    nb_ = []
                    if tt > 0:
                        nb_.append((tt - 1, 0))
                    nb_.append((tt, 3 if tt == 0 else (4 if tt == NT - 1 else 1)))
                    if tt < NT - 1:
                        nb_.append((tt + 1, 2))
                    for idx, (st_, blk) in enumerate(nb_):
                        S.op("tensor", lambda e, pp=pp, ti=ti, st_=st_, g=g, blk=blk, idx=idx, last=len(nb_) - 1: e.matmul(
                            pp[:, ti * 128:(ti + 1) * 128], lhsT=U[:, st_, g * 128:(g + 1) * 128], rhs=band[:, g, blk, :],
                            start=(idx == 0), stop=(idx == last)), reads=[bU[st_], self.bCST], writes=[ppb])
                pl, bpl = POOLED[pk % 2], bPOOLED[pk % 2]
                pk += 1
                S.op("scalar", lambda e, pl=pl, pp=pp: e.activation(out=pl, in_=pp[:, :], func=AF.Copy), reads=[ppb], writes=[bpl])
                pa, pab = self.PS[7], self.PSB[7]
                S.op("tensor", lambda e, pa=pa, g=g, pl=pl: e.matmul(pa[:, :], lhsT=Wpool[:, g * 128:(g + 1) * 128], rhs=pl, start=True, stop=True),
                     reads=[bWp, bpl], writes=[pab])
                S.op("vector", lambda e, pa=pa, g=g, mixt=mixt: e.tensor_scalar(
                    out=mixt[:, g, :], in0=pa[:, :], scalar1=self.EVB[:, j, g:g + 1], scalar2=self.EVS[:, j, g:g + 1], op0=ALU.add, op1=ALU.mult),
                    reads=[pab, self.bCST], writes=[bmx[g]])
            steps = [(h, kt) for h in range(8) for kt in range(NT)]

            def qk(i, si, qs=qs, qb=qb):
                h, kt = steps[i]
                kh, hb, hp = h // 4, (h % 2) * 64, h // 2
                ps_, psb_ = self.PS[si % 3], self.PSB[si % 3]
                S.op("tensor", lambda e: e.matmul(ps_[:, :], lhsT=KZ[:, kh * 2 + h % 2, kt * 128:(kt + 1) * 128], rhs=QT[:, hp, qs],
                                                  start=True, stop=True), reads=[bQK[kt]] + [bQK[t_] for t_ in range(qb * 4, qb * 4 + 4)], writes=[psb_])

            qk(0, si)
            for i, (h, kt) in enumerate(steps):
                kh = h // 4
                if i + 1 < len(steps):
                    qk(i + 1, si + 1)
                ps_, psb_ = self.PS[si % 3], self.PSB[si % 3]
                pt, bpt = PT[si % 3], bPT[si % 3]
                si += 1
                S.op("scalar", lambda e, ps_=ps_, pt=pt: e.activation(out=pt, in_=ps_[:, :], func=AF.Exp, scale=0.125), reads=[psb_], writes=[bpt])
                po, pob = self.PS[4 + h % 2], self.PSB[4 + h % 2]
                S.op("tensor", lambda e, po=po, kt=kt, kh=kh, pt=pt: e.matmul(po[:, :], lhsT=V[:, kt, kh, :], rhs=pt, start=(kt == 0), stop=(kt == NT - 1)),
                     reads=[bV[kt], bVones, bpt], writes=[pob])
                if kt == NT - 1:
                    rc, brc = RCP[h % 2], bRCP[h % 2]
                    S.op("vector", lambda e, po=po, rc=rc: e.reciprocal(out=rc[64:128, :], in_=po[64:128, :]), reads=[pob], writes=[brc])
                    S.op("vector", lambda e, po=po, rc=rc, h=h: e.tensor_tensor(out=ATT[0:64, h, :], in0=po[0:64, :], in1=rc[64:128, :], op=ALU.mult),
                         reads=[pob, brc], writes=[bATT[h]])
            for c in range(KC):
                pc, pcb = self.PS[6 + c % 2], self.PSB[6 + c % 2]
                for g in range(4):
                    if DBG.get("nopool"):
                        continue
                    S.op("tensor", lambda e, pc=pc, g=g, c=c, mixt=mixt: e.matmul(
                        pc[:, :], lhsT=WoutP[:, g * 1024 + c * 128: g * 1024 + c * 128 + 128], rhs=mixt[:, g, :], start=(g == 0), stop=bool(DBG.get("noatt")) and g == 3),
                        reads=[bWoP, bmx[g]], writes=[pcb])
                for h in range(8):
                    if DBG.get("noatt"):
                        continue
                    S.op("tensor", lambda e, pc=pc, h=h, c=c: e.matmul(
                        pc[:, :], lhsT=WoutA[:, h * 1024 + c * 128: h * 1024 + c * 128 + 128], rhs=ATT[0:64, h, :], start=bool(DBG.get("nopool")) and h == 0, stop=(h == 7)),
                        reads=[bWoA, bATT[h]], writes=[pcb])
                S.op("vector", lambda e, pc=pc, c=c, qs=qs: e.scalar_tensor_tensor(
                    out=self.XT[:, c, qs], in0=pc[:, :], scalar=self.MOD[:, l, 16 + c, n:n + 1], in1=self.XT[:, c, qs], op0=ALU.mult, op1=ALU.add),
                    reads=[pcb, self.bMOD, self.bXT[c][qb]], writes=[self.bXT[c][qb]])

    def emit_odd(self, l, n):
        S = self.S
        j = l // 2
        K1 = 1024
        self.emit_norm(l, 0, n, 48 * K1)
        S.barrier()
        R3 = lambda ap, a: ap.rearrange("p (a b) -> p a b", a=a)
        flat = lambda a: a.rearrange("p d t h -> p (d t h)")
        Win = self.WR[:, 0:KC * 768]
        Wout = self.WR[:, 6144:6144 + 2048]
        Wg = self.WR[:, 8192:8192 + 256]
        VT = self.view(self.WR, 16896, [NT, 2, 130], BF16)
        WK = self.view(self.WR, 25216, [2, NT, 8], F32)
        DEC = self.view(self.WR, 26240, [2, NT, 8], F32)
        HGAIN = self.view(self.WR, 27264, [256], F32)
        DEC2 = self.view(self.WR, 28288, [2, NT], F32)
        QKT = self.view(self.AR, 0, [2, SEQ], BF16)
        QS = self.view(self.AR, 8192, [2, SEQ], BF16)
        CT = self.view(self.AR, 16384, [2, NT, 130], BF16)
        T4 = self.view(self.AR, 24704, [SEQ], BF16)
        SH = 28800
        cst = self.bCST
        identb = self.cb("ident")
        selb = self.cb("selb")
        selc = self.cb("selc")
        bWin, bWout, bWg, bG, bTab, bT4 = (Buf() for _ in range(6))
        bVT = [Buf() for _ in range(NT)]
        bVone = Buf()
        rot = {"a": 0}

        def nxt(lo, n_):
            rot["a"] += 1
            k_ = lo + rot["a"] % n_
            return self.PS[k_], self.PSB[k_]

        G = self.view(self.AR, SH, [NT, 32], F32)
        GB = self.view(self.AR, SH + 2048, [32], F32)
        LOGF, II, BB, TG = (self.view(self.AR, SH + 2304 + i * 1024, [2, NT, 8], F32) for i in range(4))
        CC = self.view(self.WR, 28416, [2, NT, 8], F32)
        TABb = self.view(self.AR, SH + 2304 + 5 * 1024, [NT, 64], BF16)
        S.dma(WQ, Wg, self.dr["odWg"][j], writes=[bWg])
        S.dma("sync", GB, self.dr["odGateB"][j].partition_broadcast(128), writes=[bG])
        self.ms("gpsimd", VT[:, :, :, 128:129], 1.0, [bVone])
        pg, pgb = self.PS[0], self.PSB[0]
        for tt in range(NT):
            for kc in range(KC):
                self.mm(pg[:, tt * 32:(tt + 1) * 32], self.HT[:, kc, tt * 128:(tt + 1) * 128], Wg[:, kc * 32:(kc + 1) * 32],
                        kc == 0, kc == KC - 1, [bWg, self.bHT[tt // 4]], [pgb])
        self.tt("vector", G, R3(pg[:, :], NT), GB.unsqueeze(1).to_broadcast([128, NT, 32]), ALU.add, [pgb, bG], [bG])
        G5 = G.rearrange("p t (d i h) -> p t d i h", d=2, i=2)
        for d in range(2):
            self.act(TG[:, d], G5[:, :, d, 1, :], AF.Exp, [bG], [bTab], scale=-1.0)
            self.act(TG[:, d], TG[:, d], AF.Ln, [bTab], [bTab], bias=1.0)
            self.ts("vector", LOGF[:, d], TG[:, d], -1.0, None, ALU.mult, None, [bTab], [bTab])
            self.cp("vector", II[:, d], G5[:, :, d, 0, :], [bG], [bTab])
        pB, pBb = self.PS[1], self.PSB[1]
        pL, pLb = self.PS[2], self.PSB[2]
        for d in range(2):
            self.mm(pB[:, d * 128:(d + 1) * 128], self.cf("tri_f" if d == 0 else "tri_b"), LOGF[:, d].rearrange("p t h -> p (t h)"),
                    True, True, [bTab, cst], [pBb])
        self.mm(pL[:, 0:256], self.cf("ones"), flat(LOGF), True, True, [bTab, cst], [pLb])
        self.act(flat(BB), pB[:, 0:256], AF.Copy, [pBb], [bTab])
        self.act(flat(DEC), pL[:, 0:256], AF.Exp, [pLb], [bTab, pLb])
        self.tt("vector", flat(CC), flat(II), flat(BB), ALU.subtract, [bTab], [bTab])
        self.tt("vector", flat(TG), pL[:, 0:256], flat(CC), ALU.add, [pLb, bTab], [bTab])
        self.act(flat(WK), flat(TG), AF.Exp, [bTab], [bTab])
        for i, X in enumerate((BB, CC)):
            hi = TABb[:, :, i * 32:i * 32 + 16].rearrange("p t (d h) -> p d t h", d=2)
            lo = TABb[:, :, i * 32 + 16:i * 32 + 32].rearrange("p t (d h) -> p d t h", d=2)
            self.cp("vector", hi, X, [bTab], [bTab])
            self.tt("vector", lo, X, hi, ALU.subtract, [bTab], [bTab])
        self.ms("gpsimd", T4[64:128, :], 0.0, [bT4])
        for half in range(2):
            pp, ppb = self.PS[3 + half], self.PSB[3 + half]
            pv = pp[:, :].bitcast(BF16)
            for ti in range(8):
                tt = half * 8 + ti
                self.tr(pv[0:64, ti * 128:(ti + 1) * 128], TABb[:, tt, :], identb, [bTab, cst], [ppb])
            self.cp("vector", T4[0:64, half * 1024:(half + 1) * 1024], pv[0:64, 0:1024], [ppb], [bT4])
        S.barrier()
        if DBG.get("odd_stop") == "gate":
            return

        for hp in DBG.get("hps", range(4)):
            QKraw = self.view(self.AR, SH, [2052], BF16)
            EB = [self.view(self.AR, SH + 4104 + i * 2048, [512], F32) for i in range(2)]
            DIAG = self.view(self.AR, SH + 8200, [2, 5, 128], BF16)
            bQKraw, bDG, bHG, bDEC2 = Buf(), Buf(), Buf(), Buf()
            bEB = [Buf(), Buf()]
            bQKT = [[Buf() for _ in range(NB)] for _ in range(2)]
            bQS = [[Buf() for _ in range(NB)] for _ in range(2)]
            for hh in range(2):
                S.dma(WQ, Win[:, hh * 3072:(hh + 1) * 3072], self.dr["odWin"][j, hp, :, hh * 3072:(hh + 1) * 3072], writes=[bWin])
            S.dma(WQ, Wout, self.dr["odWout"][j, hp], writes=[bWout])
            S.dma("sync", HGAIN, self.dr["odHGain"][j, hp * 256:(hp + 1) * 256].partition_broadcast(128), writes=[bHG])
            self.ms("gpsimd", QKraw[:, 0:2], 0.0, [bQKraw])
            self.ms("gpsimd", QKraw[:, 2050:2052], 0.0, [bQKraw])
            for ci in range(2):
                fc = hp + 4 * ci
                for tap in range(5):
                    self.ts("vector", DIAG[:, ci, tap, :], identb, self.CONVW[:, j, fc, tap:tap + 1], None, ALU.mult, None, [cst], [bDG])
            for hh in range(2):
                h = 2 * hp + hh
                self.cp("vector", DEC2[hh * 64:(hh + 1) * 64, :, :], DEC[hh * 64:(hh + 1) * 64, :, :, h], [bTab], [bDEC2])
            for ci in range(2):
                fc = hp + 4 * ci
                for tb in range(NB):
                    pp, ppb = nxt(0, 6)
                    for kc in range(KC):
                        self.mm(pp[:, :], Win[:, kc * 768 + ci * 128: kc * 768 + ci * 128 + 128], self.HT[:, kc, tb * 512:(tb + 1) * 512],
                                kc == 0, kc == KC - 1, [bWin, self.bHT[tb]], [ppb])
                    self.act(QKraw[:, 2 + tb * 512: 2 + (tb + 1) * 512], pp[:, :], AF.Copy, [ppb], [bQKraw])
                for tb in range(NB):
                    pp, ppb = nxt(0, 6)
                    for tap in range(5):
                        self.mm(pp[:, :], DIAG[:, ci, tap, :], QKraw[:, tb * 512 + tap: tb * 512 + tap + 512], tap == 0, tap == 4,
                                [bDG, bQKraw], [ppb])
                    self.act(QKT[:, ci, tb * 512:(tb + 1) * 512], pp[:, :], AF.Silu, [ppb, cst], [bQKT[ci][tb]], bias=self.CONVB[:, j, fc:fc + 1])
                    if ci == 1:
                        self.ts("gpsimd", QKT[:, 1, tb * 512:(tb + 1) * 512], QKT[:, 1, tb * 512:(tb + 1) * 512], 0.125, 0.0, ALU.mult, ALU.add,
                                [bQKT[1][tb]], [bQKT[1][tb]])
            for tt in range(NT):
                pp, ppb = nxt(0, 6)
                for kc in range(KC):
                    self.mm(pp[:, 0:256], self.HT[:, kc, tt * 128:(tt + 1) * 128], Win[:, kc * 768 + 256: kc * 768 + 512],
                            kc == 0, kc == KC - 1, [bWin, self.bHT[tt // 4]], [ppb])
                self.act(VT[:, tt, :, 0:128], R3(pp[:, 0:256], 2), AF.Copy, [ppb], [bVT[tt]])
            ek = 0
            for d in range(2):
                r0 = d * 8 + 2 * hp
                selh = self.cb("selh")[:, (d * 4 + hp) * 128:(d * 4 + hp + 1) * 128]
                for tb in range(NB):
                    pp, ppb = nxt(0, 6)
                    self.mm(pp[:, :], selh, T4[:, tb * 512:(tb + 1) * 512], True, True, [bT4, cst], [ppb])
                    eb, beb = EB[ek % 2], bEB[ek % 2]
                    ek += 1
                    self.act(eb, pp[:, :], AF.Exp, [ppb], [beb])
                    self.tt("vector", QS[:, d, tb * 512:(tb + 1) * 512], QKT[:, 0, tb * 512:(tb + 1) * 512], eb, ALU.mult,
                            [bQKT[0][tb], beb], [bQS[d][tb]])
            S.barrier()
            if DBG.get("odd_stop") == "A":
                return
            KTOK = self.view(self.AR, SH, [NT, 128], BF16)
            VS = self.view(self.AR, SH + 4096, [NT, 2, 130], BF16)
            DCs = self.view(self.AR, SH + 12416, [NT, 130], F32)
            bKT = [Buf() for _ in range(NB)]
            bVS = Buf()
            bDC = [Buf() for _ in range(NT)]
            bCT = [Buf(), Buf()]
            for tb in range(NB):
                pp, ppb = nxt(0, 6)
                pv = pp[:, :].bitcast(BF16)
                for ti in range(4):
                    tt = tb * 4 + ti
                    self.tr(pv[:, ti * 128:(ti + 1) * 128], QKT[:, 1, tt * 128:(tt + 1) * 128], identb, [bQKT[1][tb], cst], [ppb])
                self.cp("vector", KTOK[:, tb * 4:(tb + 1) * 4, :], R3(pv[:, 0:512], 4), [ppb], [bKT[tb]])
            self.ms("vector", CT[:, 0, 0, :], 0.0, [bCT[0]])
            self.ms("vector", CT[:, 1, NT - 1, :], 0.0, [bCT[1]])
            for d in range(2):
                self.tt("gpsimd", VS[:, :, :, 0:129], VT[:, :, :, 0:129],
                        WK[:, d, :, 2 * hp:2 * hp + 2].unsqueeze(3).to_broadcast([128, NT, 2, 129]), ALU.mult,
                        bVT + [bVone, bTab], [bVS])
                order = list(range(NT - 1)) if d == 0 else list(range(NT - 1, 0, -1))
                for jj in order:
                    pd, pdb = nxt(6, 2)
                    for hh in range(2):
                        self.mm(pd[hh * 64:(hh + 1) * 64, 0:129], KTOK[:, jj, hh * 64:(hh + 1) * 64], VS[:, jj, hh, 0:129], True, True,
                                [bKT[jj // 4], bVS], [pdb])
                    self.act(DCs[:, jj, 0:129], pd[:, 0:129], AF.Copy, [pdb], [bDC[jj]])
                for step in range(1, len(order)):
                    jj, jp = order[step], order[step - 1]
                    self.stt(DCs[:, jj, 0:129], DCs[:, jp, 0:129], DEC2[:, d, jj:jj + 1], DCs[:, jj, 0:129], ALU.mult, ALU.add,
                             [bDC[jp], bDC[jj], bDEC2], [bDC[jj]])
                if d == 0:
                    self.act(CT[:, 0, 1:NT, 0:129], DCs[:, 0:NT - 1, 0:129], AF.Copy, bDC, [bCT[0]])
                else:
                    self.act(CT[:, 1, 0:NT - 1, 0:129], DCs[:, 1:NT, 0:129], AF.Copy, bDC, [bCT[1]])
            S.barrier()
            if DBG.get("odd_stop") == "B":
                return
            W = [self.view(self.AR, SH + i * 2048, [512], F32) for i in range(2)]
            PT = [self.view(self.AR, SH + 4096 + i * 1024, [4, 128], BF16) for i in range(2)]
            HSb = self.view(self.AR, SH + 6144, [NT, 2, 128], BF16)
            HSf = [self.view(self.AR, SH + 14336 + i * 1024, [2, 128], F32) for i in range(2)]
            DSC = self.view(self.AR, SH + 16384, [2, 16], F32)
            SIG = [self.view(self.AR, SH + 16512 + i * 1024, [256], F32) for i in range(2)]
            TMPG = [self.view(self.AR, SH + 18560 + i * 1024, [256], F32) for i in range(2)]
            GATED = [self.view(self.AR, SH + 20608 + i * 512, [256], BF16) for i in range(2)]
            GT = [self.view(self.AR, SH + 21632 + i * 2048, [2, 512], BF16) for i in range(2)]
            SQJ = self.view(self.AR, SH + 25728, [128], F32)
            bW, bPT, bHSf, bSIG, bTMPG, bGATED, bGTt, bDSC = ([Buf(), Buf()] for _ in range(8))
            bHSb = [Buf() for _ in range(NT)]
            bSQJ, bSSQ = Buf(), Buf()
            pST, pSTb = self.PS[0:2], self.PSB[0:2]
            pE, pEb = self.PS[2:4], self.PSB[2:4]
            pT, pTb = self.PS[4:8], self.PSB[4:8]

            QZ = [self.view(self.AR, SH + 26240 + i * 512, [2, 128], BF16) for i in range(2)]
            CTB = [self.view(self.AR, SH + 27264 + i * 1032, [2, 258], BF16) for i in range(2)]
            bQZ, bCTB = [Buf(), Buf()], [Buf(), Buf()]
            for i in range(2):
                self.ms("gpsimd", QZ[i], 0.0, [bQZ[i]])
                self.ms("gpsimd", CTB[i], 0.0, [bCTB[i]])

            def stA(tt):
                k_ = tt % 2
                tsl = slice(tt * 128, (tt + 1) * 128)
                tb = tt // 4
                self.cp("gpsimd", QZ[k_][0:64, 0, :], QKT[0:64, 0, tsl], [bQKT[0][tb]], [bQZ[k_]])
                self.cp("gpsimd", QZ[k_][64:128, 1, :], QKT[64:128, 0, tsl], [bQKT[0][tb]], [bQZ[k_]])
                self.cp("gpsimd", CTB[k_][0:64, :, 0:129], CT[0:64, :, tt, 0:129], [bCT[0], bCT[1]], [bCTB[k_]])
                self.cp("gpsimd", CTB[k_][64:128, :, 129:258], CT[64:128, :, tt, 0:129], [bCT[0], bCT[1]], [bCTB[k_]])
                self.mm(pST[k_][:, 0:256], QKT[:, 1, tsl], QZ[k_].rearrange("p a b -> p (a b)"), True, True, [bQKT[1][tb], bQZ[k_]], [pSTb[k_]])
                for hh in range(2):
                    for d in range(2):
                        q_ = hh * 2 + d
                        r = d * 8 + 2 * hp + hh
                        oe = pE[k_][:, q_ * 128:(q_ + 1) * 128]
                        self.mm(oe, selb[:, r:r + 1].to_broadcast([128, 128]), T4[:, tsl], True, False, [bT4, cst], [pEb[k_]])
                        self.mm(oe, identb, self.cb("neg_f" if d == 0 else "neg_b"), False, True, [cst], [pEb[k_]])
                for hh in range(2):
                    for d in range(2):
                        q_ = hh * 2 + d
                        h = 2 * hp + hh
                        self.act(W[k_][:, q_ * 128:(q_ + 1) * 128], pE[k_][:, q_ * 128:(q_ + 1) * 128], AF.Exp, [pEb[k_], bTab], [bW[k_]],
                                 bias=CC[:, d, tt, h:h + 1])
                self.tt("vector", PT[k_].rearrange("p (h d) t -> p h d t", h=2), W[k_].rearrange("p (h d t) -> p h d t", h=2, d=2),
                        pST[k_][:, 0:256].rearrange("p (h t) -> p h t", h=2).unsqueeze(2).to_broadcast([128, 2, 2, 128]), ALU.mult,
                        [bW[k_], pSTb[k_]], [bPT[k_]])

            def stB(tt):
                k_ = tt % 2
                tsl = slice(tt * 128, (tt + 1) * 128)
                tb = tt // 4
                HS, bhs = HSf[k_], bHSf[k_]
                for d in range(2):
                    pt_, ptb_ = pT[k_ * 2 + d], pTb[k_ * 2 + d]
                    for hh in range(2):
                        q_ = hh * 2 + d
                        S.op("tensor", lambda e, pt_=pt_, hh=hh, q_=q_: e.matmul(
                            pt_[:, hh * 129:(hh + 1) * 129], lhsT=PT[k_][:, q_, :], rhs=VT[:, tt, hh, 0:129], start=(hh == 0), stop=False,
                            skip_group_check=True), reads=[bPT[k_], bVT[tt], bVone], writes=[ptb_])
                    S.op("tensor", lambda e, pt_=pt_, d=d: e.matmul(
                        pt_[:, 0:258], lhsT=QS[:, d, tsl], rhs=CTB[k_][:, d, :], start=False, stop=True, skip_group_check=True),
                        reads=[bQS[d][tb], bCTB[k_]], writes=[ptb_])
                for d in range(2):
                    pt_, ptb_ = pT[k_ * 2 + d], pTb[k_ * 2 + d]
                    den = pt_[:, 128:258:129]
                    sm = DSC[:, k_, :]
                    a_, b_, r_ = sm[:, 0:2], sm[:, 2:4], sm[:, 4 + d * 2:6 + d * 2]
                    self.ts("vector", a_, den, -1.0, 1.0, ALU.mult, ALU.max, [ptb_], [bDSC[k_]])
                    self.ts("vector", b_, den, 1.0, None, ALU.max, None, [ptb_], [bDSC[k_]])
                    self.tt("vector", a_, a_, b_, ALU.max, [bDSC[k_]], [bDSC[k_]])
                    self.rcp(r_, a_, [bDSC[k_]], [bDSC[k_]])
                    for hh in range(2):
                        if d == 0:
                            self.ts("vector", HS[:, hh, :], pt_[:, hh * 129:hh * 129 + 128], r_[:, hh:hh + 1], None, ALU.mult, None,
                                    [ptb_, bDSC[k_]], [bhs])
                        else:
                            self.stt(HSb[:, tt, hh, :], pt_[:, hh * 129:hh * 129 + 128], r_[:, hh:hh + 1], HS[:, hh, :], ALU.mult, ALU.add,
                                     [ptb_, bDSC[k_], bhs], [bHSb[tt]])

            for step in range(NT + 1):
                if step < NT:
                    stA(step)
                if 0 <= step - 1 < NT:
                    stB(step - 1)
            SSQ = self.SM[:, 0:32]
            RST = self.SM[:, 32:64]
            for tt in range(NT):
                for hh in range(2):
                    self.act(SQJ, HSb[:, tt, hh, :], AF.Square, [bHSb[tt]], [bSQJ, bSSQ], accum_out=SSQ[:, tt * 2 + hh:tt * 2 + hh + 1])
            self.ts("vector", RST, SSQ, 1.0 / 128, EPS, ALU.mult, ALU.add, [bSSQ], [bSSQ])
            self.act(RST, RST, AF.Sqrt, [bSSQ], [bSSQ])
            self.rcp(RST, RST, [bSSQ], [bSSQ])
            pO, pOb = self.PS[0:2], self.PSB[0:2]
            pR, pRb = self.PS[2:4], self.PSB[2:4]
            pC, pCb = self.PS[4:6], self.PSB[4:6]

            def stC1(tt):
                k_ = tt % 2
                tsl = slice(tt * 128, (tt + 1) * 128)
                for kc in range(KC):
                    self.mm(pO[k_][:, 0:256], self.HT[:, kc, tsl], Win[:, kc * 768 + 512: kc * 768 + 768], kc == 0, kc == KC - 1,
                            [bWin, self.bHT[tt // 4]], [pOb[k_]])
                self.act(SIG[k_], pO[k_][:, 0:256], AF.Sigmoid, [pOb[k_]], [bSIG[k_]])

            def stC2(tt):
                k_ = tt % 2
                tb = tt // 4
                for hh in range(2):
                    self.stt(TMPG[k_][:, hh * 128:(hh + 1) * 128], HSb[:, tt, hh, :], RST[:, tt * 2 + hh:tt * 2 + hh + 1],
                             HGAIN[:, hh * 128:(hh + 1) * 128], ALU.mult, ALU.mult, [bHSb[tt], bSSQ, bHG], [bTMPG[k_]])
                self.tt("gpsimd", GATED[k_], TMPG[k_], SIG[k_], ALU.mult, [bTMPG[k_], bSIG[k_]], [bGATED[k_]])
                pv = pR[k_][:, 0:128].bitcast(BF16)
                for i in range(2):
                    self.tr(pv[:, i * 128:(i + 1) * 128], GATED[k_][:, i * 128:(i + 1) * 128], identb, [bGATED[k_], cst], [pRb[k_]])
                gt, bgt = GT[tb % 2], bGTt[tb % 2]
                self.act(gt[:, :, (tt % 4) * 128:(tt % 4 + 1) * 128], R3(pv[:, 0:256], 2), AF.Copy, [pRb[k_]], [bgt])
                if tt % 4 == 3:
                    qs = slice(tb * 512, (tb + 1) * 512)
                    for c in range(KC):
                        pc, pcb = pC[c % 2], pCb[c % 2]
                        for i in range(2):
                            self.mm(pc[:, :], Wout[:, i * 1024 + c * 128: i * 1024 + c * 128 + 128], gt[:, i, :], i == 0, i == 1, [bWout, bgt], [pcb])
                        self.stt(self.XT[:, c, qs], pc[:, :], self.MOD[:, l, 16 + c, n:n + 1], self.XT[:, c, qs], ALU.mult, ALU.add,
                                 [pcb, self.bMOD, self.bXT[c][tb]], [self.bXT[c][tb]])

            for step in range(NT + 1):
                if step < NT:
                    stC1(step)
                if 0 <= step - 1 < NT:
                    stC2(step - 1)
            S.barrier()


FULL_PLAN = [(k, l) for l in range(NL) for k in ("mix", "ffn")]
_PROG_CACHE = {}


def run_plan(inputs, plan, ns, bsel=None):
    shared = prep_shared(inputs)
    key = (ns, tuple(plan))
    if key not in _PROG_CACHE:
        _PROG_CACHE[key] = Prog(ns, plan)
    prog = _PROG_CACHE[key]
    ncores = NCORES if bsel is None else len(bsel)
    if bsel is None:
        bsel = [list(range(i * ns, (i + 1) * ns)) for i in range(NCORES)]
    in_maps = []
    for i in range(ncores):
        m = dict(shared)
        m.update(prep_core(inputs, bsel[i]))
        in_maps.append(m)
    res = run_bass_kernel_spmd(prog.nc, in_maps, core_ids=list(range(ncores)))
    outs = []
    for i in range(ncores):
        yT = np.asarray(res.results[i]["yT"])
        outs.append(yT.transpose(0, 3, 2, 1).reshape(len(bsel[i]), SEQ, DM))
    return np.concatenate(outs, axis=0)


def kernel(**inputs):
    inputs = {k: np.asarray(v) for k, v in inputs.items()}
    out = run_plan(inputs, FULL_PLAN, BATCH // NCORES)
    return np.ascontiguousarray(out, dtype=np.float32)
```

```python
import contextlib
import numpy as np
import concourse.bass as bass
import concourse.mybir as mybir
from concourse.bass_utils import run_bass_kernel_spmd

F32 = mybir.dt.float32
BF16 = mybir.dt.bfloat16
AF = mybir.ActivationFunctionType
ALU = mybir.AluOpType
AX = mybir.AxisListType

NCORES = 8
BATCH = 32
SEQ = 2048
DM = 1024
KC = 8
FH = 2816
HC = 22
NL = 4
EPS = 1e-6
NT = SEQ // 128
NB = SEQ // 512


class Stream:
    __slots__ = ("sem", "name", "val")

    def __init__(self, sem, name):
        self.sem = sem
        self.name = name
        self.val = 0


class Buf:
    __slots__ = ("name", "w", "r")

    def __init__(self, name=""):
        self.name = name
        self.w = None
        self.r = {}


ENGS = ("tensor", "vector", "scalar", "gpsimd", "sync")


class Sched:
    def __init__(self, nc, stack, n_dma=16, same_engine_sync=True):
        self.nc = nc
        self.q = {e: [] for e in ENGS}
        self.st = {e: Stream(stack.enter_context(nc.semaphore("s_" + e)), e) for e in ENGS}
        self.dma_pool = {}
        for q_ in ("sync", "gpsimd", "scalar"):
            self.dma_pool[q_] = [Stream(stack.enter_context(nc.semaphore("d%s%d" % (q_, i))), "d%s%d" % (q_, i)) for i in range(n_dma if q_ != "scalar" else 8)]
        self.dma_st = [s_ for q_ in self.dma_pool for s_ in self.dma_pool[q_]]
        self.dma_rr = {q_: 0 for q_ in self.dma_pool}
        self.seen = {e: {} for e in ENGS}
        self.same = same_engine_sync
        self.nins = {e: 0 for e in ENGS}

    def _need(self, eng, deps):
        seen = self.seen[eng]
        for (s, v) in deps:
            if s is self.st[eng]:
                if eng == "tensor" or eng == "sync" or not self.same:
                    continue
            if seen.get(s, 0) >= v:
                continue
            seen[s] = v
            self.q[eng].append(("w", s.sem, v))

    @staticmethod
    def _deps(reads, writes):
        deps = []
        for b in reads:
            if b.w is not None:
                deps.append(b.w)
        for b in writes:
            if b.w is not None:
                deps.append(b.w)
            for s, v in b.r.items():
                deps.append((s, v))
        return deps

    @staticmethod
    def _mark(tok, reads, writes):
        s, v = tok
        for b in reads:
            if b.r.get(s, 0) < v:
                b.r[s] = v
        for b in writes:
            b.w = tok
            b.r = {}

    def op(self, eng, fn, reads=(), writes=()):
        self._need(eng, self._deps(reads, writes))
        s = self.st[eng]
        s.val += 1
        self.q[eng].append(("i", fn, s.sem, 1))
        self.nins[eng] += 1
        self._mark((s, s.val), reads, writes)

    def dma(self, eng, out, in_, reads=(), writes=()):
        pool = self.dma_pool[eng]
        d = pool[self.dma_rr[eng]]
        self.dma_rr[eng] = (self.dma_rr[eng] + 1) % len(pool)
        deps = self._deps(reads, writes)
        if d.val:
            deps.append((d, d.val))
        self._need(eng, deps)
        d.val += 16
        self.q[eng].append(("i", lambda e, o=out, i=in_: e.dma_start(out=o, in_=i), d.sem, 16))
        self.nins[eng] += 1
        self._mark((d, d.val), reads, writes)

    def barrier(self):
        deps = [(s, s.val) for s in self.dma_st if s.val] + [(self.st[e], self.st[e].val) for e in ENGS if self.st[e].val]
        for e in ENGS:
            self._need(e, deps)

    def finish(self):
        deps = [(s, s.val) for s in self.dma_st if s.val] + [(self.st[e], self.st[e].val) for e in ENGS if e != "sync" and self.st[e].val]
        self._need("sync", deps)

    def emit(self):
        nc = self.nc
        with nc.Block() as block:
            for e in ENGS:
                items = self.q[e]

                def body(engobj, items=items):
                    for it in items:
                        if it[0] == "w":
                            engobj.wait_ge(it[1], it[2])
                        else:
                            it[1](engobj).then_inc(it[2], it[3])
                getattr(block, e)(body)


POOL_W = (2, 4, 8, 16)


def _band_blocks():
    out = np.zeros((4, 5, 128, 128), np.float32)
    for g, w in enumerate(POOL_W):
        P = np.zeros((SEQ, SEQ), np.float64)
        def row(t):
            lo = min(max(t - w // 2, 0), SEQ)
            hi = min(max(t + w - w // 2, 0), SEQ)
            r = np.zeros(SEQ)
            r[lo:hi] = 1.0 / (hi - lo)
            r[t] -= 1.0
            return r
        def blk(ti, si):
            b = np.zeros((128, 128))
            for tt in range(128):
                r = row(ti * 128 + tt)
                b[:, tt] = r[si * 128:(si + 1) * 128]
            return b
        out[g, 0] = blk(5, 4)
        out[g, 1] = blk(5, 5)
        out[g, 2] = blk(5, 6)
        out[g, 3] = blk(0, 0)
        out[g, 4] = blk(NT - 1, NT - 1)
    return out


def _rope_tables():
    t = np.arange(SEQ)
    row = (t // 64).astype(np.float32)
    col = (t % 64).astype(np.float32)
    n_freq = 16
    inv_freq = (np.float32(10000.0) ** (-np.arange(n_freq, dtype=np.float32) / n_freq)).astype(np.float32)
    ang = np.concatenate([row[:, None] * inv_freq, col[:, None] * inv_freq], axis=-1).astype(np.float32)
    cos = np.cos(ang).astype(np.float32)
    sin = np.sin(ang).astype(np.float32)
    cos = cos.reshape(NT, 128, 32).transpose(1, 0, 2)
    sin = sin.reshape(NT, 128, 32).transpose(1, 0, 2)
    return np.ascontiguousarray(cos), np.ascontiguousarray(sin)


CF = {}
_off = 0
for _name, _n in (("cos", NT * 32), ("sin", NT * 32), ("tri_f", 128), ("tri_b", 128), ("ones", 128), ("ident", 128)):
    CF[_name] = (_off, _n)
    _off += _n
NCF = _off
CB = {}
_off = 0
for _name, _n in (("ident", 128), ("ones", 128), ("band", 4 * 5 * 128), ("neg_f", 128), ("neg_b", 128), ("selb", 16), ("selc", 16), ("selh", 8 * 128)):
    CB[_name] = (_off, _n)
    _off += _n
NCB = _off


def _const_tables():
    cf = np.zeros((128, NCF), np.float32)
    cos, sin = _rope_tables()
    cf[:, CF["cos"][0]:CF["cos"][0] + NT * 32] = cos.reshape(128, -1)
    cf[:, CF["sin"][0]:CF["sin"][0] + NT * 32] = sin.reshape(128, -1)
    k = np.arange(128)[:, None]
    j = np.arange(128)[None, :]
    cf[:, CF["tri_f"][0]:CF["tri_f"][0] + 128] = (k <= j)
    cf[:, CF["tri_b"][0]:CF["tri_b"][0] + 128] = (k >= j)
    cf[:, CF["ones"][0]:CF["ones"][0] + 128] = 1.0
    cf[:, CF["ident"][0]:CF["ident"][0] + 128] = np.eye(128)
    cb = np.zeros((128, NCB), np.float32)
    cb[:, CB["ident"][0]:CB["ident"][0] + 128] = np.eye(128)
    cb[:, CB["ones"][0]:CB["ones"][0] + 128] = 1.0
    bb = _band_blocks()
    cb[:, CB["band"][0]:CB["band"][0] + 4 * 5 * 128] = bb.transpose(2, 0, 1, 3).reshape(128, -1)
    cb[:, CB["neg_f"][0]:CB["neg_f"][0] + 128] = np.where(k <= j, 0.0, -30000.0)
    cb[:, CB["neg_b"][0]:CB["neg_b"][0] + 128] = np.where(k >= j, 0.0, -30000.0)
    kk = np.arange(128)[:, None]
    rr = np.arange(16)[None, :]
    cb[:, CB["selb"][0]:CB["selb"][0] + 16] = ((kk == rr) | (kk == 16 + rr))
    cb[:, CB["selc"][0]:CB["selc"][0] + 16] = ((kk == 32 + rr) | (kk == 48 + rr))
    for d_ in range(2):
        for hp_ in range(4):
            r0 = d_ * 8 + 2 * hp_
            o_ = CB["selh"][0] + (d_ * 4 + hp_) * 128
            kcol = np.arange(128)[:, 0] if False else np.arange(128)
            cb[:, o_:o_ + 64] = ((kcol == r0) | (kcol == 16 + r0))[:, None]
            cb[:, o_ + 64:o_ + 128] = ((kcol == r0 + 1) | (kcol == 17 + r0))[:, None]
    return cf, cb


def prep_shared(inp):
    f = lambda a: np.ascontiguousarray(a, dtype=np.float32)
    d = {}
    aw = inp["ada_w"].reshape(NL, KC, 128, 8, 768)
    d["adaW"] = f(aw.transpose(0, 3, 2, 1, 4).reshape(NL, 8, 128, KC * 768))
    d["adaB"] = f(inp["ada_b"].reshape(NL, 48, 128).transpose(2, 0, 1))
    w1 = inp["ffn_w1"].reshape(NL, KC, 128, 11, 2, 128)
    w3 = inp["ffn_w3"].reshape(NL, KC, 128, 11, 2, 128)
    w13 = np.stack([w1, w3], axis=0)
    d["w13"] = f(w13.transpose(1, 4, 3, 0, 5, 2, 6).reshape(NL, 11, 128, 4096))
    w2 = inp["ffn_w2"].reshape(NL, HC, 128, 4, 2, 128)
    d["w2"] = f(w2.transpose(0, 3, 2, 4, 1, 5).reshape(NL, 4, 128, 2 * HC * 128))
    d["evWin"] = f(inp["ev_w_in"].reshape(2, KC, 128, 1280).transpose(0, 2, 1, 3).reshape(2, 128, KC * 1280))
    d["evWpool"] = f(inp["ev_w_pool"].transpose(0, 2, 1, 3).reshape(2, 128, 512))
    d["evBpool"] = f(inp["ev_b_pool"].transpose(2, 0, 1))
    d["evPscale"] = f(inp["ev_pool_scale"].reshape(2, 4, 128).transpose(2, 0, 1))
    d["evGain"] = f(np.concatenate([np.tile(inp["ev_q_gain"], (1, 8)), np.tile(inp["ev_k_gain"], (1, 2))], axis=1))
    wo = inp["ev_w_out"]
    d["evWoutP"] = f(wo[:, :512].reshape(2, 4, 128, DM).transpose(0, 2, 1, 3).reshape(2, 128, 4 * DM))
    d["evWoutA"] = f(wo[:, 512:].reshape(2, 8, 64, DM).transpose(0, 2, 1, 3).reshape(2, 64, 8 * DM))
    wi = inp["od_w_in"]
    per_hp = []
    for hp in range(4):
        cols = np.concatenate([np.arange(hp * 128, hp * 128 + 128), 512 + np.arange(hp * 128, hp * 128 + 128),
                               1024 + np.arange(hp * 256, hp * 256 + 256), 2048 + np.arange(hp * 256, hp * 256 + 256)])
        per_hp.append(wi[:, :, cols])
    whp = np.stack(per_hp, axis=1).reshape(2, 4, KC, 128, 768)
    d["odWin"] = f(whp.transpose(0, 1, 3, 2, 4).reshape(2, 4, 128, KC * 768))
    d["odWg"] = f(wi[:, :, 3072:3104].reshape(2, KC, 128, 32).transpose(0, 2, 1, 3).reshape(2, 128, KC * 32))
    d["odConvW"] = f(inp["od_conv_w"].reshape(2, 5, 8, 128).transpose(3, 0, 2, 1))
    d["odConvB"] = f(inp["od_conv_b"].reshape(2, 8, 128).transpose(2, 0, 1))
    d["odGateB"] = f(inp["od_gate_b"])
    d["odHGain"] = f(inp["od_head_gain"].reshape(2, 1024))
    d["odWout"] = f(inp["od_w_out"].reshape(2, 4, 2, 128, DM).transpose(0, 1, 3, 2, 4).reshape(2, 4, 128, 2 * DM))
    cf, cb = _const_tables()
    d["cstF"] = cf
    d["cstB"] = cb
    return d


def prep_core(inp, bidx):
    x = inp["x"][bidx]
    ns = len(bidx)
    xT = np.ascontiguousarray(x.reshape(ns, SEQ, KC, 128).transpose(0, 3, 2, 1), dtype=np.float32)
    c = inp["c"][bidx]
    cT = np.ascontiguousarray(c.reshape(ns, KC, 128).transpose(2, 1, 0).reshape(128, KC * ns), dtype=np.float32)
    return {"xT": xT, "cT": cT}


SHARED_SHAPES = {
    "adaW": [NL, 8, 128, KC * 768], "adaB": [128, NL, 48], "w13": [NL, 11, 128, 4096], "w2": [NL, 4, 128, 2 * HC * 128],
    "evWin": [2, 128, KC * 1280], "evWpool": [2, 128, 512], "evBpool": [128, 2, 4], "evPscale": [128, 2, 4],
    "evGain": [2, 640], "evWoutP": [2, 128, 4 * DM], "evWoutA": [2, 64, 8 * DM],
    "odWin": [2, 4, 128, KC * 768], "odWg": [2, 128, KC * 32], "odConvW": [128, 2, 8, 5], "odConvB": [128, 2, 8],
    "odGateB": [2, 32], "odHGain": [2, 1024], "odWout": [2, 4, 128, 2 * DM],
    "cstF": [128, NCF], "cstB": [128, NCB],
}


DBG = {}
WQ = "gpsimd"
ARENA_BYTES = 58 * 1024
WREG_BYTES = 34560


class Prog:
    def __init__(self, ns, plan, first_layer_mod=True):
        self.ns = ns
        self.plan = plan
        nc = self.nc = bass.Bass("TRN2", target_bir_lowering=False)
        self.dr = {}
        self.dr["xT"] = nc.dram_tensor("xT", [ns, 128, KC, SEQ], F32, kind="ExternalInput").ap()
        self.dr["cT"] = nc.dram_tensor("cT", [128, KC * ns], F32, kind="ExternalInput").ap()
        for k, sh in SHARED_SHAPES.items():
            self.dr[k] = nc.dram_tensor(k, sh, F32, kind="ExternalInput").ap()
        self.dr["yT"] = nc.dram_tensor("yT", [ns, 128, KC, SEQ], F32, kind="ExternalOutput").ap()
        with contextlib.ExitStack() as st:
            self.stack = st
            self.S = Sched(nc, st)
            self._alloc()
            self._emit_all()
            self.S.finish()
            self.S.emit()

    def sb(self, name, shape, dt):
        return self.stack.enter_context(self.nc.sbuf_tensor(name, shape, dt))

    def view(self, region, boff, shape, dt):
        esz = 4 if dt == F32 else 2
        n = int(np.prod(shape))
        assert boff % 4 == 0
        a = region[:, boff // 2: boff // 2 + n * esz // 2]
        if dt == F32:
            a = a.bitcast(F32)
        if len(shape) == 2:
            a = a.rearrange("p (a b) -> p a b", a=shape[0])
        elif len(shape) == 3:
            a = a.rearrange("p (a b c) -> p a b c", a=shape[0], b=shape[1])
        return a

    def _alloc(self):
        nc = self.nc
        ns = self.ns
        self.XT = self.sb("XT", [128, KC, SEQ], F32)
        self.HTr = self.sb("HTr", [128, KC * SEQ], BF16)
        self.HT = self.HTr[:, :].rearrange("p (a b) -> p a b", a=KC)
        self.AR = self.sb("AR", [128, ARENA_BYTES // 2], BF16)
        self.WR = self.sb("WR", [128, WREG_BYTES // 2], BF16)
        self.CSTF = self.sb("CSTF", [128, NCF], F32)
        self.CSTB = self.sb("CSTB", [128, NCB], BF16)
        self.MOD = self.sb("MOD", [128, NL, 48, ns], F32)
        self.MODP1 = self.sb("MODP1", [128, NL, 2, 8, ns], F32)
        self.ADAB = self.sb("ADAB", [128, NL, 48], F32)
        self.COND = self.sb("COND", [128, KC * ns], F32)
        self.EVB = self.sb("EVB", [128, 2, 4], F32)
        self.EVS = self.sb("EVS", [128, 2, 4], F32)
        self.SM = self.sb("SM", [128, 64], F32)
        self.EPSC = self.sb("EPSC", [128, 2], F32)
        self.CONVW = self.sb("CONVW", [128, 2, 8, 5], F32)
        self.CONVB = self.sb("CONVB", [128, 2, 8], F32)
        self.PS = [self.stack.enter_context(nc.psum_tensor("ps%d" % i, [128, 512], F32)) for i in range(8)]
        self.PSB = [Buf("ps%d" % i) for i in range(8)]
        self.bXT = [[Buf("xt%d_%d" % (c, tb)) for tb in range(NB)] for c in range(KC)]
        self.bHT = [Buf("ht%d" % tb) for tb in range(NB)]
        self.bCST = Buf("cst")
        self.bMOD = Buf("mod")

    def cf(self, name):
        o, n = CF[name]
        return self.CSTF[:, o:o + n]

    def cb(self, name):
        o, n = CB[name]
        return self.CSTB[:, o:o + n]

    def mm(self, out, lhsT, rhs, start, stop, reads, writes):
        self.S.op("tensor", lambda e: e.matmul(out, lhsT=lhsT, rhs=rhs, start=start, stop=stop), reads=reads, writes=writes)

    def tr(self, out, in_, ident, reads, writes):
        self.S.op("tensor", lambda e: e.transpose(out=out, in_=in_, identity=ident), reads=reads, writes=writes)

    def act(self, out, in_, func, reads, writes, **kw):
        self.S.op("scalar", lambda e: e.activation(out=out, in_=in_, func=func, **kw), reads=reads, writes=writes)

    def tt(self, eng, out, in0, in1, op, reads, writes):
        self.S.op(eng, lambda e: e.tensor_tensor(out=out, in0=in0, in1=in1, op=op), reads=reads, writes=writes)

    def ts(self, eng, out, in0, s1, s2, op0, op1, reads, writes):
        if op1 is None:
            self.S.op(eng, lambda e: e.tensor_scalar(out=out, in0=in0, scalar1=s1, scalar2=None, op0=op0), reads=reads, writes=writes)
        else:
            self.S.op(eng, lambda e: e.tensor_scalar(out=out, in0=in0, scalar1=s1, scalar2=s2, op0=op0, op1=op1), reads=reads, writes=writes)

    def stt(self, out, in0, scalar, in1, op0, op1, reads, writes):
        self.S.op("vector", lambda e: e.scalar_tensor_tensor(out=out, in0=in0, scalar=scalar, in1=in1, op0=op0, op1=op1), reads=reads, writes=writes)

    def cp(self, eng, out, in_, reads, writes):
        self.S.op(eng, lambda e: e.tensor_copy(out=out, in_=in_), reads=reads, writes=writes)

    def ms(self, eng, ap, val, writes):
        self.S.op(eng, lambda e: e.memset(ap, val), writes=writes)

    def rcp(self, out, in_, reads, writes):
        self.S.op("vector", lambda e: e.reciprocal(out=out, in_=in_), reads=reads, writes=writes)

    def _emit_all(self):
        S = self.S
        S.dma("sync", self.CSTF[:, :], self.dr["cstF"], writes=[self.bCST])
        S.dma(WQ, self.CSTB[:, :], self.dr["cstB"], writes=[self.bCST])
        S.dma("sync", self.EVB[:, :, :], self.dr["evBpool"], writes=[self.bCST])
        S.dma("sync", self.EVS[:, :, :], self.dr["evPscale"], writes=[self.bCST])
        S.dma("sync", self.CONVW[:, :, :, :], self.dr["odConvW"], writes=[self.bCST])
        S.dma("sync", self.CONVB[:, :, :], self.dr["odConvB"], writes=[self.bCST])
        S.op("vector", lambda e: e.memset(self.EPSC[:, :], EPS), writes=[self.bCST])
        for c in range(KC):
            S.dma("scalar", self.XT[:, c, :], self.dr["xT"][0, :, c, :], writes=self.bXT[c])
        self.emit_ada()
        S.barrier()
        for n in range(self.ns):
            for c in range(KC):
                if n > 0:
                    S.dma("sync", self.XT[:, c, :], self.dr["xT"][n, :, c, :], writes=self.bXT[c])
            for (kind, l) in self.plan:
                if kind == "ffn":
                    self.emit_ffn(l, n)
                elif kind == "mix" and l % 2 == 0:
                    self.emit_even(l, n)
                elif kind == "mix":
                    self.emit_odd(l, n)
                S.barrier()
            for c in range(KC):
                S.dma("sync", self.dr["yT"][n, :, c, :], self.XT[:, c, :], reads=self.bXT[c])

    def emit_ada(self):
        S, ns = self.S, self.ns
        S.dma("sync", self.COND[:, :], self.dr["cT"], writes=[self.bMOD])
        S.dma("sync", self.ADAB[:, :, :], self.dr["adaB"], writes=[self.bMOD])
        S.op("scalar", lambda e: e.activation(out=self.COND[:, :], in_=self.COND[:, :], func=AF.Silu), reads=[self.bMOD], writes=[self.bMOD])
        AW = [self.view(self.AR, i * 24576, [KC * 768], F32) for i in range(2)]
        bAW = [Buf("aw0"), Buf("aw1")]
        ps = self.PS[0]
        pb = self.PSB[0]
        k = 0
        for l in range(NL):
            for q in range(8):
                w, bw = AW[k % 2], bAW[k % 2]
                k += 1
                S.dma("sync", w, self.dr["adaW"][l, q], writes=[bw])
                for fcl in range(6):
                    fc = q * 6 + fcl
                    for kc in range(KC):
                        S.op("tensor", lambda e, w=w, fcl=fcl, kc=kc, fc=fc: e.matmul(
                            ps[:, fc * ns:(fc + 1) * ns], lhsT=w[:, kc * 768 + fcl * 128: kc * 768 + fcl * 128 + 128],
                            rhs=self.COND[:, kc * ns:(kc + 1) * ns], start=(kc == 0), stop=(kc == KC - 1)),
                            reads=[bw, self.bMOD], writes=[pb])
            S.op("vector", lambda e, l=l: e.tensor_tensor(
                out=self.MOD[:, l, :, :], in0=ps[:, 0:48 * ns].rearrange("p (a b) -> p a b", a=48),
                in1=self.ADAB[:, l, :].unsqueeze(2).to_broadcast([128, 48, ns]), op=ALU.add),
                reads=[pb, self.bMOD], writes=[self.bMOD])
            for i, base in enumerate((8, 32)):
                S.op("vector", lambda e, l=l, i=i, base=base: e.tensor_scalar(
                    out=self.MODP1[:, l, i, :, :], in0=self.MOD[:, l, base:base + 8, :], scalar1=1.0, scalar2=None, op0=ALU.add),
                    reads=[self.bMOD], writes=[self.bMOD])

    def emit_norm(self, l, which, n, scr_off):
        S = self.S
        SQ = [self.view(self.AR, scr_off + i * 1024, [512], BF16) for i in range(2)]
        RS = [self.view(self.AR, scr_off + 2048 + i * 2048, [512], F32) for i in range(2)]
        TMP = [self.view(self.AR, scr_off + 6144 + i * 2048, [512], F32) for i in range(2)]
        bSQ = [Buf(), Buf()]
        bRS = [Buf(), Buf()]
        bTMP = [Buf(), Buf()]
        ones = self.cb("ones")
        shb = 0 if which == 0 else 24

        def part1(tb):
            ts = slice(tb * 512, (tb + 1) * 512)
            ps, pb = self.PS[6 + tb % 2], self.PSB[6 + tb % 2]
            rs, brs = RS[tb % 2], bRS[tb % 2]
            for c in range(KC):
                self.act(SQ[c % 2], self.XT[:, c, ts], AF.Square, [self.bXT[c][tb]], [bSQ[c % 2]])
                self.mm(ps[:, :], ones, SQ[c % 2], c == 0, c == KC - 1, [bSQ[c % 2], self.bCST], [pb])
            self.act(rs, ps[:, :], AF.Ln, [pb, self.bCST], [brs], scale=1.0 / DM, bias=self.EPSC[:, 0:1])
            self.act(rs, rs, AF.Exp, [brs], [brs], scale=-0.5)

        def part2(tb):
            ts = slice(tb * 512, (tb + 1) * 512)
            rs, brs = RS[tb % 2], bRS[tb % 2]
            for c in range(KC):
                self.tt("vector", TMP[c % 2], self.XT[:, c, ts], rs, ALU.mult, [self.bXT[c][tb], brs], [bTMP[c % 2]])
                self.act(self.HT[:, c, ts], TMP[c % 2], AF.Identity, [bTMP[c % 2], self.bMOD], [self.bHT[tb]],
                         bias=self.MOD[:, l, shb + c, n:n + 1], scale=self.MODP1[:, l, which, c, n:n + 1])

        part1(0)
        for tb in range(NB):
            if tb + 1 < NB:
                part1(tb + 1)
            part2(tb)

    def emit_ffn(self, l, n):
        S = self.S
        self.emit_norm(l, 1, n, 48 * 1024)
        AT = self.view(self.AR, 0, [HC, 1024], BF16)
        SG = [self.view(self.AR, 44 * 1024 + i * 2048, [512], F32) for i in range(2)]
        bAT = [[Buf() for _ in range(2)] for _ in range(HC)]
        bSG = [Buf(), Buf()]
        WS = [self.WR[:, i * 5632:(i + 1) * 5632] for i in range(3)]
        bWS = [Buf(), Buf(), Buf()]
        wk = 0
        pk = 0
        for half in range(2):
            for jp in range(11):
                ws, bw = WS[wk % 3], bWS[wk % 3]
                wk += 1
                S.dma(WQ, ws[:, 0:4096], self.dr["w13"][l, jp], writes=[bw])
                for jj in range(2):
                    j = 2 * jp + jj
                    for tb2 in range(2):
                        tb = half * 2 + tb2
                        ts = slice(tb * 512, (tb + 1) * 512)
                        pg, pgb = self.PS[pk % 2], self.PSB[pk % 2]
                        pu, pub = self.PS[2 + pk % 2], self.PSB[2 + pk % 2]
                        sg, bsg = SG[pk % 2], bSG[pk % 2]
                        pk += 1
                        for (pp, ppb, wsel) in ((pg, pgb, 0), (pu, pub, 1)):
                            for kc in range(KC):
                                o = (wsel * 2 + jj) * 1024 + kc * 128
                                S.op("tensor", lambda e, pp=pp, o=o, kc=kc, ts=ts, ws=ws: e.matmul(
                                    pp[:, :], lhsT=ws[:, o:o + 128], rhs=self.HT[:, kc, ts], start=(kc == 0), stop=(kc == KC - 1)),
                                    reads=[bw, self.bHT[tb]], writes=[ppb])
                        S.op("scalar", lambda e, pg=pg, sg=sg: e.activation(out=sg, in_=pg[:, :], func=AF.Silu), reads=[pgb], writes=[bsg])
                        S.op("vector", lambda e, pu=pu, sg=sg, j=j, tb2=tb2: e.tensor_tensor(
                            out=AT[:, j, tb2 * 512:(tb2 + 1) * 512], in0=pu[:, :], in1=sg, op=ALU.mult),
                            reads=[pub, bsg], writes=[bAT[j][tb2]])
            for cp in range(4):
                ws, bw = WS[wk % 3], bWS[wk % 3]
                wk += 1
                S.dma(WQ, ws[:, 0:5632], self.dr["w2"][l, cp], writes=[bw])
                for cc in range(2):
                    c = 2 * cp + cc
                    for tb2 in range(2):
                        tb = half * 2 + tb2
                        ts = slice(tb * 512, (tb + 1) * 512)
                        po, pob = self.PS[4 + pk % 2], self.PSB[4 + pk % 2]
                        pk += 1
                        for j in range(HC):
                            o = cc * HC * 128 + j * 128
                            S.op("tensor", lambda e, po=po, o=o, j=j, tb2=tb2, ws=ws: e.matmul(
                                po[:, :], lhsT=ws[:, o:o + 128], rhs=AT[:, j, tb2 * 512:(tb2 + 1) * 512], start=(j == 0), stop=(j == HC - 1)),
                                reads=[bw, bAT[j][tb2]], writes=[pob])
                        S.op("vector", lambda e, po=po, c=c, ts=ts: e.scalar_tensor_tensor(
                            out=self.XT[:, c, ts], in0=po[:, :], scalar=self.MOD[:, l, 40 + c, n:n + 1], in1=self.XT[:, c, ts],
                            op0=ALU.mult, op1=ALU.add),
                            reads=[pob, self.bMOD, self.bXT[c][tb]], writes=[self.bXT[c][tb]])

    def emit_even(self, l, n):
        S = self.S
        j = l // 2
        K1 = 1024
        self.emit_norm(l, 0, n, 48 * K1)
        S.barrier()
        U = self.view(self.AR, 0, [NT, 512], BF16)
        QT = self.view(self.AR, 16 * K1, [4, SEQ], BF16)
        KZ = self.view(self.AR, 32 * K1, [4, SEQ], BF16)
        V = self.view(self.AR, 48 * K1, [NT, 2, 128], BF16)
        T1 = self.view(self.WR, 20480, [320], F32)
        T2 = self.view(self.WR, 20480 + 1280, [320], F32)
        QROT = self.view(self.WR, 20480 + 2560, [640], BF16)
        SQ = self.view(self.WR, 28160, [640], F32)
        QN = self.view(self.WR, 28160 + 2560, [640], F32)
        KD = self.view(self.WR, 33280, [512], BF16)
        Win = self.WR[:, 0:KC * 1280]
        Wpool = self.WR[:, 12288:12288 + 512]
        GAIN = self.WR[:, 12800:12800 + 1280].bitcast(F32)
        bWin, bWp, bGain = Buf(), Buf(), Buf()
        bU = [Buf() for _ in range(NT)]
        bQK = [Buf() for _ in range(NT)]
        bV = [Buf() for _ in range(NT)]
        bVones = Buf()
        bSQ, bQN, bT1, bT2, bQROT, bKD, bSM = (Buf() for _ in range(7))
        for hh in range(2):
            S.dma(WQ, Win[:, hh * 5120:(hh + 1) * 5120], self.dr["evWin"][j, :, hh * 5120:(hh + 1) * 5120], writes=[bWin])
        S.dma(WQ, Wpool, self.dr["evWpool"][j], writes=[bWp])
        S.dma("sync", GAIN, self.dr["evGain"][j].partition_broadcast(128), writes=[bGain])
        S.op("gpsimd", lambda e: e.memset(V[:, :, :, 64:128], 1.0), writes=[bVones])
        S.op("gpsimd", lambda e: e.memset(KD, 0.0), writes=[bKD])
        ident = self.cb("ident")
        SSQ = self.SM[:, 0:10]
        RST = self.SM[:, 16:26]
        cosv = self.cf("cos").rearrange("p (a b) -> p a b", a=NT)
        sinv = self.cf("sin").rearrange("p (a b) -> p a b", a=NT)
        for tt in range(NT):
            tsl = slice(tt * 128, (tt + 1) * 128)
            tb = tt // 4
            pu, pub = self.PS[tt % 2], self.PSB[tt % 2]
            pq, pqb = self.PS[2 + tt % 2], self.PSB[2 + tt % 2]
            pkv, pkvb = self.PS[4 + tt % 2], self.PSB[4 + tt % 2]
            for (pp, ppb, c0, cn) in ((pu, pub, 0, 512), (pq, pqb, 512, 512), (pkv, pkvb, 1024, 256)):
                for kc in range(KC):
                    S.op("tensor", lambda e, pp=pp, kc=kc, tsl=tsl, c0=c0, cn=cn: e.matmul(
                        pp[:, 0:cn], lhsT=self.HT[:, kc, tsl], rhs=Win[:, kc * 1280 + c0: kc * 1280 + c0 + cn],
                        start=(kc == 0), stop=(kc == KC - 1)), reads=[bWin, self.bHT[tb]], writes=[ppb])
            S.op("scalar", lambda e, pu=pu, tt=tt: e.activation(out=U[:, tt, :], in_=pu[:, :], func=AF.Copy), reads=[pub], writes=[bU[tt]])
            S.op("scalar", lambda e, pkv=pkv, tt=tt: e.activation(
                out=V[:, tt, :, 0:64], in_=pkv[:, 128:256].rearrange("p (a b) -> p a b", a=2), func=AF.Copy),
                reads=[pkvb], writes=[bV[tt]])
            S.op("scalar", lambda e, pq=pq: e.activation(out=SQ[:, 0:512], in_=pq[:, :], func=AF.Square), reads=[pqb], writes=[bSQ])
            S.op("scalar", lambda e, pkv=pkv: e.activation(out=SQ[:, 512:640], in_=pkv[:, 0:128], func=AF.Square), reads=[pkvb], writes=[bSQ])
            S.op("vector", lambda e: e.tensor_reduce(out=SSQ, in_=SQ.rearrange("p (a b) -> p a b", a=10), axis=AX.X, op=ALU.add),
                 reads=[bSQ], writes=[bSM])
            S.op("vector", lambda e: e.tensor_scalar(out=RST, in0=SSQ, scalar1=1.0 / 64, scalar2=EPS, op0=ALU.mult, op1=ALU.add),
                 reads=[bSM], writes=[bSM])
            S.op("scalar", lambda e: e.activation(out=RST, in_=RST, func=AF.Sqrt), reads=[bSM], writes=[bSM])
            S.op("vector", lambda e: e.reciprocal(out=RST, in_=RST), reads=[bSM], writes=[bSM])
            S.op("vector", lambda e, pq=pq: e.tensor_tensor(
                out=QN[:, 0:512].rearrange("p (a b) -> p a b", a=8), in0=pq[:, :].rearrange("p (a b) -> p a b", a=8),
                in1=RST[:, 0:8].unsqueeze(2).to_broadcast([128, 8, 64]), op=ALU.mult), reads=[pqb, bSM], writes=[bQN])
            S.op("vector", lambda e, pkv=pkv: e.tensor_tensor(
                out=QN[:, 512:640].rearrange("p (a b) -> p a b", a=2), in0=pkv[:, 0:128].rearrange("p (a b) -> p a b", a=2),
                in1=RST[:, 8:10].unsqueeze(2).to_broadcast([128, 2, 64]), op=ALU.mult), reads=[pkvb, bSM], writes=[bQN])
            S.op("vector", lambda e: e.tensor_tensor(out=QN, in0=QN, in1=GAIN, op=ALU.mult), reads=[bQN, bGain], writes=[bQN])
            x1 = QN[:, 0:640:2].rearrange("p (a b) -> p a b", a=10)
            x2 = QN[:, 1:640:2].rearrange("p (a b) -> p a b", a=10)
            o1 = QROT[:, 0:640:2].rearrange("p (a b) -> p a b", a=10)
            o2 = QROT[:, 1:640:2].rearrange("p (a b) -> p a b", a=10)
            t1 = T1.rearrange("p (a b) -> p a b", a=10)
            t2 = T2.rearrange("p (a b) -> p a b", a=10)
            cs = cosv[:, tt, :].unsqueeze(1).to_broadcast([128, 10, 32])
            sn = sinv[:, tt, :].unsqueeze(1).to_broadcast([128, 10, 32])
            S.op("vector", lambda e, cs=cs: e.tensor_tensor(out=t1, in0=x1, in1=cs, op=ALU.mult), reads=[bQN, self.bCST], writes=[bT1])
            S.op("gpsimd", lambda e, sn=sn: e.tensor_tensor(out=t2, in0=x2, in1=sn, op=ALU.mult), reads=[bQN, self.bCST], writes=[bT2])
            S.op("vector", lambda e: e.tensor_tensor(out=o1, in0=t1, in1=t2, op=ALU.subtract), reads=[bT1, bT2], writes=[bQROT])
            S.op("vector", lambda e, sn=sn: e.tensor_tensor(out=t1, in0=x1, in1=sn, op=ALU.mult), reads=[bQN, self.bCST], writes=[bT1])
            S.op("gpsimd", lambda e, cs=cs: e.tensor_tensor(out=t2, in0=x2, in1=cs, op=ALU.mult), reads=[bQN, self.bCST], writes=[bT2])
            S.op("vector", lambda e: e.tensor_tensor(out=o2, in0=t1, in1=t2, op=ALU.add), reads=[bT1, bT2], writes=[bQROT])
            KD3 = KD.rearrange("p (a x) -> p a x", a=2)
            krot = QROT[:, 512:640].rearrange("p (a c) -> p a c", a=2)
            S.op("gpsimd", lambda e: e.tensor_copy(out=KD3[:, :, 0:64], in_=krot), reads=[bQROT], writes=[bKD])
            S.op("gpsimd", lambda e: e.tensor_copy(out=KD3[:, :, 192:256], in_=krot), reads=[bQROT], writes=[bKD])
            ptr, ptrb = self.PS[6 + tt % 2], self.PSB[6 + tt % 2]
            ptv = ptr[:, :].bitcast(BF16)
            for hp in range(4):
                S.op("tensor", lambda e, hp=hp, ptv=ptv: e.transpose(out=ptv[:, hp * 128:(hp + 1) * 128], in_=QROT[:, hp * 128:(hp + 1) * 128], identity=ident),
                     reads=[bQROT, self.bCST], writes=[ptrb])
            for v_ in range(4):
                S.op("tensor", lambda e, v_=v_, ptv=ptv: e.transpose(out=ptv[:, 512 + v_ * 128:512 + (v_ + 1) * 128], in_=KD[:, v_ * 128:(v_ + 1) * 128], identity=ident),
                     reads=[bKD, self.bCST], writes=[ptrb])
            S.op("scalar", lambda e, ptv=ptv, tsl=tsl: e.activation(out=QT[:, :, tsl], in_=ptv[:, 0:512].rearrange("p (a b) -> p a b", a=4), func=AF.Copy),
                 reads=[ptrb], writes=[bQK[tt]])
            S.op("scalar", lambda e, ptv=ptv, tsl=tsl: e.activation(out=KZ[:, :, tsl], in_=ptv[:, 512:1024].rearrange("p (a b) -> p a b", a=4), func=AF.Copy),
                 reads=[ptrb], writes=[bQK[tt]])
        S.barrier()
        PT = [self.view(self.HTr, i * K1, [512], BF16) for i in range(3)]
        RCP = [self.view(self.HTr, 4 * K1 + i * 2 * K1, [512], F32) for i in range(2)]
        POOLED = [self.view(self.HTr, 8 * K1 + i * K1, [512], BF16) for i in range(2)]
        MIXT = [self.view(self.HTr, 10 * K1 + i * 4 * K1, [4, 512], BF16) for i in range(2)]
        ATT = self.view(self.HTr, 18 * K1, [8, 512], BF16)
        bPT = [Buf() for _ in range(3)]
        bRCP = [Buf(), Buf()]
        bPOOLED = [Buf(), Buf()]
        bMIXT = [[Buf() for _ in range(4)] for _ in range(2)]
        bATT = [Buf() for _ in range(8)]
        WoutP = self.WR[:, 0:4096]
        WoutA = self.WR[0:64, 4096:4096 + 8192]
        bWoP, bWoA = Buf(), Buf()
        S.dma(WQ, WoutP, self.dr["evWoutP"][j], writes=[bWoP])
        S.dma(WQ, WoutA, self.dr["evWoutA"][j], writes=[bWoA])
        band = self.cb("band").rearrange("p (g r t) -> p g r t", g=4, r=5)
        pk = 0
        si = 0
        for qb in range(NB):
            qs = slice(qb * 512, (qb + 1) * 512)
            mixt, bmx = MIXT[qb % 2], bMIXT[qb % 2]
            for g in range(4):
                pp, ppb = self.PS[6], self.PSB[6]
                for ti in range(4):
                    tt = qb * 4 + ti
                    nb_ = []
                    if tt > 0:
                        nb_.append((tt - 1, 0))
                    nb_.append((tt, 3 if tt == 0 else (4 if tt == NT - 1 else 1)))
                    if tt < NT - 1:
                        nb_.append((tt + 1, 2))
                    for idx, (st_, blk) in enumerate(nb_):
                        S.op("tensor", lambda e, pp=pp, ti=ti, st_=st_, g=g, blk=blk, idx=idx, last=len(nb_) - 1: e.matmul(
                            pp[:, ti * 128:(ti + 1) * 128], lhsT=U[:, st_, g * 128:(g + 1) * 128], rhs=band[:, g, blk, :],
                            start=(idx == 0), stop=(idx == last)), reads=[bU[st_], self.bCST], writes=[ppb])
                pl, bpl = POOLED[pk % 2], bPOOLED[pk % 2]
                pk += 1
                S.op("scalar", lambda e, pl=pl, pp=pp: e.activation(out=pl, in_=pp[:, :], func=AF.Copy), reads=[ppb], writes=[bpl])
                pa, pab = self.PS[7], self.PSB[7]
                S.op("tensor", lambda e, pa=pa, g=g, pl=pl: e.matmul(pa[:, :], lhsT=Wpool[:, g * 128:(g + 1) * 128], rhs=pl, start=True, stop=True),
                     reads=[bWp, bpl], writes=[pab])
                S.op("vector", lambda e, pa=pa, g=g, mixt=mixt: e.tensor_scalar(
                    out=mixt[:, g, :], in0=pa[:, :], scalar1=self.EVB[:, j, g:g + 1], scalar2=self.EVS[:, j, g:g + 1], op0=ALU.add, op1=ALU.mult),
                    reads=[pab, self.bCST], writes=[bmx[g]])
            steps = [(h, kt) for h in range(8) for kt in range(NT)]

            def qk(i, si, qs=qs, qb=qb):
                h, kt = steps[i]
                kh, hb, hp = h // 4, (h % 2) * 64, h // 2
                ps_, psb_ = self.PS[si % 3], self.PSB[si % 3]
                S.op("tensor", lambda e: e.matmul(ps_[:, :], lhsT=KZ[:, kh * 2 + h % 2, kt * 128:(kt + 1) * 128], rhs=QT[:, hp, qs],
                                                  start=True, stop=True), reads=[bQK[kt]] + [bQK[t_] for t_ in range(qb * 4, qb * 4 + 4)], writes=[psb_])

            qk(0, si)
            for i, (h, kt) in enumerate(steps):
                kh = h // 4
                if i + 1 < len(steps):
                    qk(i + 1, si + 1)
                ps_, psb_ = self.PS[si % 3], self.PSB[si % 3]
                pt, bpt = PT[si % 3], bPT[si % 3]
                si += 1
                S.op("scalar", lambda e, ps_=ps_, pt=pt: e.activation(out=pt, in_=ps_[:, :], func=AF.Exp, scale=0.125), reads=[psb_], writes=[bpt])
                po, pob = self.PS[4 + h % 2], self.PSB[4 + h % 2]
                S.op("tensor", lambda e, po=po, kt=kt, kh=kh, pt=pt: e.matmul(po[:, :], lhsT=V[:, kt, kh, :], rhs=pt, start=(kt == 0), stop=(kt == NT - 1)),
                     reads=[bV[kt], bVones, bpt], writes=[pob])
                if kt == NT - 1:
                    rc, brc = RCP[h % 2], bRCP[h % 2]
                    S.op("vector", lambda e, po=po, rc=rc: e.reciprocal(out=rc[64:128, :], in_=po[64:128, :]), reads=[pob], writes=[brc])
                    S.op("vector", lambda e, po=po, rc=rc, h=h: e.tensor_tensor(out=ATT[0:64, h, :], in0=po[0:64, :], in1=rc[64:128, :], op=ALU.mult),
                         reads=[pob, brc], writes=[bATT[h]])
            for c in range(KC):
                pc, pcb = self.PS[6 + c % 2], self.PSB[6 + c % 2]
                for g in range(4):
                    if DBG.get("nopool"):
                        continue
                    S.op("tensor", lambda e, pc=pc, g=g, c=c, mixt=mixt: e.matmul(
                        pc[:, :], lhsT=WoutP[:, g * 1024 + c * 128: g * 1024 + c * 128 + 128], rhs=mixt[:, g, :], start=(g == 0), stop=bool(DBG.get("noatt")) and g == 3),
                        reads=[bWoP, bmx[g]], writes=[pcb])
                for h in range(8):
                    if DBG.get("noatt"):
                        continue
                    S.op("tensor", lambda e, pc=pc, h=h, c=c: e.matmul(
                        pc[:, :], lhsT=WoutA[:, h * 1024 + c * 128: h * 1024 + c * 128 + 128], rhs=ATT[0:64, h, :], start=bool(DBG.get("nopool")) and h == 0, stop=(h == 7)),
                        reads=[bWoA, bATT[h]], writes=[pcb])
                S.op("vector", lambda e, pc=pc, c=c, qs=qs: e.scalar_tensor_tensor(
                    out=self.XT[:, c, qs], in0=pc[:, :], scalar=self.MOD[:, l, 16 + c, n:n + 1], in1=self.XT[:, c, qs], op0=ALU.mult, op1=ALU.add),
                    reads=[pcb, self.bMOD, self.bXT[c][qb]], writes=[self.bXT[c][qb]])

    def emit_odd(self, l, n):
        S = self.S
        j = l // 2
        K1 = 1024
        self.emit_norm(l, 0, n, 48 * K1)
        S.barrier()
        R3 = lambda ap, a: ap.rearrange("p (a b) -> p a b", a=a)
        flat = lambda a: a.rearrange("p d t h -> p (d t h)")
        Win = self.WR[:, 0:KC * 768]
        Wout = self.WR[:, 6144:6144 + 2048]
        Wg = self.WR[:, 8192:8192 + 256]
        VT = self.view(self.WR, 16896, [NT, 2, 130], BF16)
        WK = self.view(self.WR, 25216, [2, NT, 8], F32)
        DEC = self.view(self.WR, 26240, [2, NT, 8], F32)
        HGAIN = self.view(self.WR, 27264, [256], F32)
        DEC2 = self.view(self.WR, 28288, [2, NT], F32)
        QKT = self.view(self.AR, 0, [2, SEQ], BF16)
        QS = self.view(self.AR, 8192, [2, SEQ], BF16)
        CT = self.view(self.AR, 16384, [2, NT, 130], BF16)
        T4 = self.view(self.AR, 24704, [SEQ], BF16)
        SH = 28800
        cst = self.bCST
        identb = self.cb("ident")
        selb = self.cb("selb")
        selc = self.cb("selc")
        bWin, bWout, bWg, bG, bTab, bT4 = (Buf() for _ in range(6))
        bVT = [Buf() for _ in range(NT)]
        bVone = Buf()
        rot = {"a": 0}

        def nxt(lo, n_):
            rot["a"] += 1
            k_ = lo + rot["a"] % n_
            return self.PS[k_], self.PSB[k_]

        G = self.view(self.AR, SH, [NT, 32], F32)
        GB = self.view(self.AR, SH + 2048, [32], F32)
        LOGF, II, BB, TG = (self.view(self.AR, SH + 2304 + i * 1024, [2, NT, 8], F32) for i in range(4))
        CC = self.view(self.WR, 28416, [2, NT, 8], F32)
        TABb = self.view(self.AR, SH + 2304 + 5 * 1024, [NT, 64], BF16)
        S.dma(WQ, Wg, self.dr["odWg"][j], writes=[bWg])
        S.dma("sync", GB, self.dr["odGateB"][j].partition_broadcast(128), writes=[bG])
        self.ms("gpsimd", VT[:, :, :, 128:129], 1.0, [bVone])
        pg, pgb = self.PS[0], self.PSB[0]
        for tt in range(NT):
            for kc in range(KC):
                self.mm(pg[:, tt * 32:(tt + 1) * 32], self.HT[:, kc, tt * 128:(tt + 1) * 128], Wg[:, kc * 32:(kc + 1) * 32],
                        kc == 0, kc == KC - 1, [bWg, self.bHT[tt // 4]], [pgb])
        self.tt("vector", G, R3(pg[:, :], NT), GB.unsqueeze(1).to_broadcast([128, NT, 32]), ALU.add, [pgb, bG], [bG])
        G5 = G.rearrange("p t (d i h) -> p t d i h", d=2, i=2)
        for d in range(2):
            self.act(TG[:, d], G5[:, :, d, 1, :], AF.Exp, [bG], [bTab], scale=-1.0)
            self.act(TG[:, d], TG[:, d], AF.Ln, [bTab], [bTab], bias=1.0)
            self.ts("vector", LOGF[:, d], TG[:, d], -1.0, None, ALU.mult, None, [bTab], [bTab])
            self.cp("vector", II[:, d], G5[:, :, d, 0, :], [bG], [bTab])
        pB, pBb = self.PS[1], self.PSB[1]
        pL, pLb = self.PS[2], self.PSB[2]
        for d in range(2):
            self.mm(pB[:, d * 128:(d + 1) * 128], self.cf("tri_f" if d == 0 else "tri_b"), LOGF[:, d].rearrange("p t h -> p (t h)"),
                    True, True, [bTab, cst], [pBb])
        self.mm(pL[:, 0:256], self.cf("ones"), flat(LOGF), True, True, [bTab, cst], [pLb])
        self.act(flat(BB), pB[:, 0:256], AF.Copy, [pBb], [bTab])
        self.act(flat(DEC), pL[:, 0:256], AF.Exp, [pLb], [bTab, pLb])
        self.tt("vector", flat(CC), flat(II), flat(BB), ALU.subtract, [bTab], [bTab])
        self.tt("vector", flat(TG), pL[:, 0:256], flat(CC), ALU.add, [pLb, bTab], [bTab])
        self.act(flat(WK), flat(TG), AF.Exp, [bTab], [bTab])
        for i, X in enumerate((BB, CC)):
            hi = TABb[:, :, i * 32:i * 32 + 16].rearrange("p t (d h) -> p d t h", d=2)
            lo = TABb[:, :, i * 32 + 16:i * 32 + 32].rearrange("p t (d h) -> p d t h", d=2)
            self.cp("vector", hi, X, [bTab], [bTab])
            self.tt("vector", lo, X, hi, ALU.subtract, [bTab], [bTab])
        self.ms("gpsimd", T4[64:128, :], 0.0, [bT4])
        for half in range(2):
            pp, ppb = self.PS[3 + half], self.PSB[3 + half]
            pv = pp[:, :].bitcast(BF16)
            for ti in range(8):
                tt = half * 8 + ti
                self.tr(pv[0:64, ti * 128:(ti + 1) * 128], TABb[:, tt, :], identb, [bTab, cst], [ppb])
            self.cp("vector", T4[0:64, half * 1024:(half + 1) * 1024], pv[0:64, 0:1024], [ppb], [bT4])
        S.barrier()
        if DBG.get("odd_stop") == "gate":
            return

        for hp in DBG.get("hps", range(4)):
            QKraw = self.view(self.AR, SH, [2052], BF16)
            EB = [self.view(self.AR, SH + 4104 + i * 2048, [512], F32) for i in range(2)]
            DIAG = self.view(self.AR, SH + 8200, [2, 5, 128], BF16)
            bQKraw, bDG, bHG, bDEC2 = Buf(), Buf(), Buf(), Buf()
            bEB = [Buf(), Buf()]
            bQKT = [[Buf() for _ in range(NB)] for _ in range(2)]
            bQS = [[Buf() for _ in range(NB)] for _ in range(2)]
            for hh in range(2):
                S.dma(WQ, Win[:, hh * 3072:(hh + 1) * 3072], self.dr["odWin"][j, hp, :, hh * 3072:(hh + 1) * 3072], writes=[bWin])
            S.dma(WQ, Wout, self.dr["odWout"][j, hp], writes=[bWout])
            S.dma("sync", HGAIN, self.dr["odHGain"][j, hp * 256:(hp + 1) * 256].partition_broadcast(128), writes=[bHG])
            self.ms("gpsimd", QKraw[:, 0:2], 0.0, [bQKraw])
            self.ms("gpsimd", QKraw[:, 2050:2052], 0.0, [bQKraw])
            for ci in range(2):
                fc = hp + 4 * ci
                for tap in range(5):
                    self.ts("vector", DIAG[:, ci, tap, :], identb, self.CONVW[:, j, fc, tap:tap + 1], None, ALU.mult, None, [cst], [bDG])
            for hh in range(2):
                h = 2 * hp + hh
                self.cp("vector", DEC2[hh * 64:(hh + 1) * 64, :, :], DEC[hh * 64:(hh + 1) * 64, :, :, h], [bTab], [bDEC2])
            for ci in range(2):
                fc = hp + 4 * ci
                for tb in range(NB):
                    pp, ppb = nxt(0, 6)
                    for kc in range(KC):
                        self.mm(pp[:, :], Win[:, kc * 768 + ci * 128: kc * 768 + ci * 128 + 128], self.HT[:, kc, tb * 512:(tb + 1) * 512],
                                kc == 0, kc == KC - 1, [bWin, self.bHT[tb]], [ppb])
                    self.act(QKraw[:, 2 + tb * 512: 2 + (tb + 1) * 512], pp[:, :], AF.Copy, [ppb], [bQKraw])
                for tb in range(NB):
                    pp, ppb = nxt(0, 6)
                    for tap in range(5):
                        self.mm(pp[:, :], DIAG[:, ci, tap, :], QKraw[:, tb * 512 + tap: tb * 512 + tap + 512], tap == 0, tap == 4,
                                [bDG, bQKraw], [ppb])
                    self.act(QKT[:, ci, tb * 512:(tb + 1) * 512], pp[:, :], AF.Silu, [ppb, cst], [bQKT[ci][tb]], bias=self.CONVB[:, j, fc:fc + 1])
                    if ci == 1:
                        self.ts("gpsimd", QKT[:, 1, tb * 512:(tb + 1) * 512], QKT[:, 1, tb * 512:(tb + 1) * 512], 0.125, 0.0, ALU.mult, ALU.add,
                                [bQKT[1][tb]], [bQKT[1][tb]])
            for tt in range(NT):
                pp, ppb = nxt(0, 6)
                for kc in range(KC):
                    self.mm(pp[:, 0:256], self.HT[:, kc, tt * 128:(tt + 1) * 128], Win[:, kc * 768 + 256: kc * 768 + 512],
                            kc == 0, kc == KC - 1, [bWin, self.bHT[tt // 4]], [ppb])
                self.act(VT[:, tt, :, 0:128], R3(pp[:, 0:256], 2), AF.Copy, [ppb], [bVT[tt]])
            ek = 0
            for d in range(2):
                r0 = d * 8 + 2 * hp
                selh = self.cb("selh")[:, (d * 4 + hp) * 128:(d * 4 + hp + 1) * 128]
                for tb in range(NB):
                    pp, ppb = nxt(0, 6)
                    self.mm(pp[:, :], selh, T4[:, tb * 512:(tb + 1) * 512], True, True, [bT4, cst], [ppb])
                    eb, beb = EB[ek % 2], bEB[ek % 2]
                    ek += 1
                    self.act(eb, pp[:, :], AF.Exp, [ppb], [beb])
                    self.tt("vector", QS[:, d, tb * 512:(tb + 1) * 512], QKT[:, 0, tb * 512:(tb + 1) * 512], eb, ALU.mult,
                            [bQKT[0][tb], beb], [bQS[d][tb]])
            S.barrier()
            if DBG.get("odd_stop") == "A":
                return
            KTOK = self.view(self.AR, SH, [NT, 128], BF16)
            VS = self.view(self.AR, SH + 4096, [NT, 2, 130], BF16)
            DCs = self.view(self.AR, SH + 12416, [NT, 130], F32)
            bKT = [Buf() for _ in range(NB)]
            bVS = Buf()
            bDC = [Buf() for _ in range(NT)]
            bCT = [Buf(), Buf()]
            for tb in range(NB):
                pp, ppb = nxt(0, 6)
                pv = pp[:, :].bitcast(BF16)
                for ti in range(4):
                    tt = tb * 4 + ti
                    self.tr(pv[:, ti * 128:(ti + 1) * 128], QKT[:, 1, tt * 128:(tt + 1) * 128], identb, [bQKT[1][tb], cst], [ppb])
                self.cp("vector", KTOK[:, tb * 4:(tb + 1) * 4, :], R3(pv[:, 0:512], 4), [ppb], [bKT[tb]])
            self.ms("vector", CT[:, 0, 0, :], 0.0, [bCT[0]])
            self.ms("vector", CT[:, 1, NT - 1, :], 0.0, [bCT[1]])
            for d in range(2):
                self.tt("gpsimd", VS[:, :, :, 0:129], VT[:, :, :, 0:129],
                        WK[:, d, :, 2 * hp:2 * hp + 2].unsqueeze(3).to_broadcast([128, NT, 2, 129]), ALU.mult,
                        bVT + [bVone, bTab], [bVS])
                order = list(range(NT - 1)) if d == 0 else list(range(NT - 1, 0, -1))
                for jj in order:
                    pd, pdb = nxt(6, 2)
                    for hh in range(2):
                        self.mm(pd[hh * 64:(hh + 1) * 64, 0:129], KTOK[:, jj, hh * 64:(hh + 1) * 64], VS[:, jj, hh, 0:129], True, True,
                                [bKT[jj // 4], bVS], [pdb])
                    self.act(DCs[:, jj, 0:129], pd[:, 0:129], AF.Copy, [pdb], [bDC[jj]])
                for step in range(1, len(order)):
                    jj, jp = order[step], order[step - 1]
                    self.stt(DCs[:, jj, 0:129], DCs[:, jp, 0:129], DEC2[:, d, jj:jj + 1], DCs[:, jj, 0:129], ALU.mult, ALU.add,
                             [bDC[jp], bDC[jj], bDEC2], [bDC[jj]])
                if d == 0:
                    self.act(CT[:, 0, 1:NT, 0:129], DCs[:, 0:NT - 1, 0:129], AF.Copy, bDC, [bCT[0]])
                else:
                    self.act(CT[:, 1, 0:NT - 1, 0:129], DCs[:, 1:NT, 0:129], AF.Copy, bDC, [bCT[1]])
            S.barrier()
            if DBG.get("odd_stop") == "B":
                return
            W = [self.view(self.AR, SH + i * 2048, [512], F32) for i in range(2)]
            PT = [self.view(self.AR, SH + 4096 + i * 1024, [4, 128], BF16) for i in range(2)]
            HSb = self.view(self.AR, SH + 6144, [NT, 2, 128], BF16)
            HSf = [self.view(self.AR, SH + 14336 + i * 1024, [2, 128], F32) for i in range(2)]
            DSC = self.view(self.AR, SH + 16384, [2, 16], F32)
            SIG = [self.view(self.AR, SH + 16512 + i * 1024, [256], F32) for i in range(2)]
            TMPG = [self.view(self.AR, SH + 18560 + i * 1024, [256], F32) for i in range(2)]
            GATED = [self.view(self.AR, SH + 20608 + i * 512, [256], BF16) for i in range(2)]
            GT = [self.view(self.AR, SH + 21632 + i * 2048, [2, 512], BF16) for i in range(2)]
            SQJ = self.view(self.AR, SH + 25728, [128], F32)
            bW, bPT, bHSf, bSIG, bTMPG, bGATED, bGTt, bDSC = ([Buf(), Buf()] for _ in range(8))
            bHSb = [Buf() for _ in range(NT)]
            bSQJ, bSSQ = Buf(), Buf()
            pST, pSTb = self.PS[0:2], self.PSB[0:2]
            pE, pEb = self.PS[2:4], self.PSB[2:4]
            pT, pTb = self.PS[4:8], self.PSB[4:8]

            QZ = [self.view(self.AR, SH + 26240 + i * 512, [2, 128], BF16) for i in range(2)]
            CTB = [self.view(self.AR, SH + 27264 + i * 1032, [2, 258], BF16) for i in range(2)]
            bQZ, bCTB = [Buf(), Buf()], [Buf(), Buf()]
            for i in range(2):
                self.ms("gpsimd", QZ[i], 0.0, [bQZ[i]])
                self.ms("gpsimd", CTB[i], 0.0, [bCTB[i]])

            def stA(tt):
                k_ = tt % 2
                tsl = slice(tt * 128, (tt + 1) * 128)
                tb = tt // 4
                self.cp("gpsimd", QZ[k_][0:64, 0, :], QKT[0:64, 0, tsl], [bQKT[0][tb]], [bQZ[k_]])
                self.cp("gpsimd", QZ[k_][64:128, 1, :], QKT[64:128, 0, tsl], [bQKT[0][tb]], [bQZ[k_]])
                self.cp("gpsimd", CTB[k_][0:64, :, 0:129], CT[0:64, :, tt, 0:129], [bCT[0], bCT[1]], [bCTB[k_]])
                self.cp("gpsimd", CTB[k_][64:128, :, 129:258], CT[64:128, :, tt, 0:129], [bCT[0], bCT[1]], [bCTB[k_]])
                self.mm(pST[k_][:, 0:256], QKT[:, 1, tsl], QZ[k_].rearrange("p a b -> p (a b)"), True, True, [bQKT[1][tb], bQZ[k_]], [pSTb[k_]])
                for hh in range(2):
                    for d in range(2):
                        q_ = hh * 2 + d
                        r = d * 8 + 2 * hp + hh
                        oe = pE[k_][:, q_ * 128:(q_ + 1) * 128]
                        self.mm(oe, selb[:, r:r + 1].to_broadcast([128, 128]), T4[:, tsl], True, False, [bT4, cst], [pEb[k_]])
                        self.mm(oe, identb, self.cb("neg_f" if d == 0 else "neg_b"), False, True, [cst], [pEb[k_]])
                for hh in range(2):
                    for d in range(2):
                        q_ = hh * 2 + d
                        h = 2 * hp + hh
                        self.act(W[k_][:, q_ * 128:(q_ + 1) * 128], pE[k_][:, q_ * 128:(q_ + 1) * 128], AF.Exp, [pEb[k_], bTab], [bW[k_]],
                                 bias=CC[:, d, tt, h:h + 1])
                self.tt("vector", PT[k_].rearrange("p (h d) t -> p h d t", h=2), W[k_].rearrange("p (h d t) -> p h d t", h=2, d=2),
                        pST[k_][:, 0:256].rearrange("p (h t) -> p h t", h=2).unsqueeze(2).to_broadcast([128, 2, 2, 128]), ALU.mult,
                        [bW[k_], pSTb[k_]], [bPT[k_]])

            def stB(tt):
                k_ = tt % 2
                tsl = slice(tt * 128, (tt + 1) * 128)
                tb = tt // 4
                HS, bhs = HSf[k_], bHSf[k_]
                for d in range(2):
                    pt_, ptb_ = pT[k_ * 2 + d], pTb[k_ * 2 + d]
                    for hh in range(2):
                        q_ = hh * 2 + d
                        S.op("tensor", lambda e, pt_=pt_, hh=hh, q_=q_: e.matmul(
                            pt_[:, hh * 129:(hh + 1) * 129], lhsT=PT[k_][:, q_, :], rhs=VT[:, tt, hh, 0:129], start=(hh == 0), stop=False,
                            skip_group_check=True), reads=[bPT[k_], bVT[tt], bVone], writes=[ptb_])
                    S.op("tensor", lambda e, pt_=pt_, d=d: e.matmul(
                        pt_[:, 0:258], lhsT=QS[:, d, tsl], rhs=CTB[k_][:, d, :], start=False, stop=True, skip_group_check=True),
                        reads=[bQS[d][tb], bCTB[k_]], writes=[ptb_])
                for d in range(2):
                    pt_, ptb_ = pT[k_ * 2 + d], pTb[k_ * 2 + d]
                    den = pt_[:, 128:258:129]
                    sm = DSC[:, k_, :]
                    a_, b_, r_ = sm[:, 0:2], sm[:, 2:4], sm[:, 4 + d * 2:6 + d * 2]
                    self.ts("vector", a_, den, -1.0, 1.0, ALU.mult, ALU.max, [ptb_], [bDSC[k_]])
                    self.ts("vector", b_, den, 1.0, None, ALU.max, None, [ptb_], [bDSC[k_]])
                    self.tt("vector", a_, a_, b_, ALU.max, [bDSC[k_]], [bDSC[k_]])
                    self.rcp(r_, a_, [bDSC[k_]], [bDSC[k_]])
                    for hh in range(2):
                        if d == 0:
                            self.ts("vector", HS[:, hh, :], pt_[:, hh * 129:hh * 129 + 128], r_[:, hh:hh + 1], None, ALU.mult, None,
                                    [ptb_, bDSC[k_]], [bhs])
                        else:
                            self.stt(HSb[:, tt, hh, :], pt_[:, hh * 129:hh * 129 + 128], r_[:, hh:hh + 1], HS[:, hh, :], ALU.mult, ALU.add,
                                     [ptb_, bDSC[k_], bhs], [bHSb[tt]])

            for step in range(NT + 1):
                if step < NT:
                    stA(step)
                if 0 <= step - 1 < NT:
                    stB(step - 1)
            SSQ = self.SM[:, 0:32]
            RST = self.SM[:, 32:64]
            for tt in range(NT):
                for hh in range(2):
                    self.act(SQJ, HSb[:, tt, hh, :], AF.Square, [bHSb[tt]], [bSQJ, bSSQ], accum_out=SSQ[:, tt * 2 + hh:tt * 2 + hh + 1])
            self.ts("vector", RST, SSQ, 1.0 / 128, EPS, ALU.mult, ALU.add, [bSSQ], [bSSQ])
            self.act(RST, RST, AF.Sqrt, [bSSQ], [bSSQ])
            self.rcp(RST, RST, [bSSQ], [bSSQ])
            pO, pOb = self.PS[0:2], self.PSB[0:2]
            pR, pRb = self.PS[2:4], self.PSB[2:4]
            pC, pCb = self.PS[4:6], self.PSB[4:6]

            def stC1(tt):
                k_ = tt % 2
                tsl = slice(tt * 128, (tt + 1) * 128)
                for kc in range(KC):
                    self.mm(pO[k_][:, 0:256], self.HT[:, kc, tsl], Win[:, kc * 768 + 512: kc * 768 + 768], kc == 0, kc == KC - 1,
                            [bWin, self.bHT[tt // 4]], [pOb[k_]])
                self.act(SIG[k_], pO[k_][:, 0:256], AF.Sigmoid, [pOb[k_]], [bSIG[k_]])

            def stC2(tt):
                k_ = tt % 2
                tb = tt // 4
                for hh in range(2):
                    self.stt(TMPG[k_][:, hh * 128:(hh + 1) * 128], HSb[:, tt, hh, :], RST[:, tt * 2 + hh:tt * 2 + hh + 1],
                             HGAIN[:, hh * 128:(hh + 1) * 128], ALU.mult, ALU.mult, [bHSb[tt], bSSQ, bHG], [bTMPG[k_]])
                self.tt("gpsimd", GATED[k_], TMPG[k_], SIG[k_], ALU.mult, [bTMPG[k_], bSIG[k_]], [bGATED[k_]])
                pv = pR[k_][:, 0:128].bitcast(BF16)
                for i in range(2):
                    self.tr(pv[:, i * 128:(i + 1) * 128], GATED[k_][:, i * 128:(i + 1) * 128], identb, [bGATED[k_], cst], [pRb[k_]])
                gt, bgt = GT[tb % 2], bGTt[tb % 2]
                self.act(gt[:, :, (tt % 4) * 128:(tt % 4 + 1) * 128], R3(pv[:, 0:256], 2), AF.Copy, [pRb[k_]], [bgt])
                if tt % 4 == 3:
                    qs = slice(tb * 512, (tb + 1) * 512)
                    for c in range(KC):
                        pc, pcb = pC[c % 2], pCb[c % 2]
                        for i in range(2):
                            self.mm(pc[:, :], Wout[:, i * 1024 + c * 128: i * 1024 + c * 128 + 128], gt[:, i, :], i == 0, i == 1, [bWout, bgt], [pcb])
                        self.stt(self.XT[:, c, qs], pc[:, :], self.MOD[:, l, 16 + c, n:n + 1], self.XT[:, c, qs], ALU.mult, ALU.add,
                                 [pcb, self.bMOD, self.bXT[c][tb]], [self.bXT[c][tb]])

            for step in range(NT + 1):
                if step < NT:
                    stC1(step)
                if 0 <= step - 1 < NT:
                    stC2(step - 1)
            S.barrier()


FULL_PLAN = [(k, l) for l in range(NL) for k in ("mix", "ffn")]
_PROG_CACHE = {}


def run_plan(inputs, plan, ns, bsel=None):
    shared = prep_shared(inputs)
    key = (ns, tuple(plan))
    if key not in _PROG_CACHE:
        _PROG_CACHE[key] = Prog(ns, plan)
    prog = _PROG_CACHE[key]
    ncores = NCORES if bsel is None else len(bsel)
    if bsel is None:
        bsel = [list(range(i * ns, (i + 1) * ns)) for i in range(NCORES)]
    in_maps = []
    for i in range(ncores):
        m = dict(shared)
        m.update(prep_core(inputs, bsel[i]))
        in_maps.append(m)
    res = run_bass_kernel_spmd(prog.nc, in_maps, core_ids=list(range(ncores)))
    outs = []
    for i in range(ncores):
        yT = np.asarray(res.results[i]["yT"])
        outs.append(yT.transpose(0, 3, 2, 1).reshape(len(bsel[i]), SEQ, DM))
    return np.concatenate(outs, axis=0)


def kernel(**inputs):
    inputs = {k: np.asarray(v) for k, v in inputs.items()}
    out = run_plan(inputs, FULL_PLAN, BATCH // NCORES)
    return np.ascontiguousarray(out, dtype=np.float32)
```
